# Optimizing a Trainium2 kernel written in Bass

```python
import jax, jax.numpy as jnp
from jax import lax
import numpy as np

D_MODEL = 1024
BATCH = 16
SEQ = 2048
DEPTH = 4

GRID_W = 64
CTX_LEN = 256
HEAD_DIM = 64
BRANCH_W = D_MODEL // 4
MIX_W = 4 * BRANCH_W
H_A = BRANCH_W // HEAD_DIM
G_D = BRANCH_W // HEAD_DIM
QKV_CONV = 3
SHORT_CONV = 3
CONF_CONV = 31
GDN_CHUNK = 64
GDN_CHUNK_LOG2 = 6
MLP_CHUNK = 128
EPS = 1e-6

A_COLS = 3 * BRANCH_W + 4 * H_A
B_COLS = 3 * BRANCH_W
C_COLS = 2 * BRANCH_W
D_COLS = 2 * BRANCH_W
IN_COLS = A_COLS + B_COLS + C_COLS + D_COLS + MIX_W

kernel_name = 'hybrid_parallel_group_flow_block'


def rmsnorm(x, g):
    xf = x.astype(jnp.float32)
    y = xf * lax.rsqrt(jnp.mean(xf * xf, axis=-1, keepdims=True) + EPS)
    return (y * g.astype(jnp.float32)).astype(x.dtype)


def layernorm(x, g, b):
    xf = x.astype(jnp.float32)
    mu = jnp.mean(xf, axis=-1, keepdims=True)
    xc = xf - mu
    var = jnp.mean(xc * xc, axis=-1, keepdims=True)
    y = xc * lax.rsqrt(var + EPS) * g.astype(jnp.float32) + b.astype(jnp.float32)
    return y.astype(x.dtype)


def l2norm(x):
    return x * lax.rsqrt(jnp.sum(x * x, axis=-1, keepdims=True) + EPS)


def dwconv(x, w):
    k = w.shape[0]
    return lax.conv_general_dilated(
        x, w[:, None, :].astype(x.dtype), window_strides=(1,),
        padding=[(k // 2, k // 2)], dimension_numbers=('NWC', 'WIO', 'NWC'),
        feature_group_count=x.shape[-1])


def to_col_major(x):
    bsz, t, ch = x.shape
    rows = t // GRID_W
    return x.reshape(bsz, rows, GRID_W, ch).transpose(0, 2, 1, 3).reshape(bsz, t, ch)


def from_col_major(x):
    bsz, t, ch = x.shape
    rows = t // GRID_W
    return x.reshape(bsz, GRID_W, rows, ch).transpose(0, 2, 1, 3).reshape(bsz, t, ch)


def gdn_chunked(q, k, v, g, beta, s0):
    bsz, t, h, dk = q.shape
    n = t // GDN_CHUNK

    def chunks(a):
        a = a.reshape((bsz, n, GDN_CHUNK) + a.shape[2:])
        return jnp.moveaxis(a, 3, 2)

    qc, kc, vc, gc, bc = (chunks(a) for a in (q, k, v, g, beta))
    gcum = jnp.cumsum(gc, axis=-1)
    idx = jnp.arange(GDN_CHUNK)
    incl = idx[:, None] >= idx[None, :]
    strict = idx[:, None] > idx[None, :]
    decay = jnp.exp(jnp.where(incl, gcum[..., :, None] - gcum[..., None, :], -jnp.inf))
    kb = kc * bc[..., None]
    lmat = -jnp.where(strict, jnp.einsum('bnhid,bnhjd->bnhij', kb, kc) * decay, 0.0)
    tinv = jnp.eye(GDN_CHUNK, dtype=jnp.float32) + lmat
    power = lmat
    for _ in range(GDN_CHUNK_LOG2 - 1):
        power = power @ power
        tinv = tinv + tinv @ power
    u = tinv @ (vc * bc[..., None])
    w = tinv @ (kb * jnp.exp(gcum)[..., None])
    attn = jnp.einsum('bnhid,bnhjd->bnhij', qc, kc) * decay
    qg = qc * jnp.exp(gcum)[..., None]
    kdec = kc * jnp.exp(gcum[..., -1:] - gcum)[..., None]
    glast = jnp.exp(gcum[..., -1])

    def step(s, xs):
        qg_i, kdec_i, u_i, w_i, attn_i, gl_i = xs
        v_new = u_i - w_i @ s
        o_i = qg_i @ s + attn_i @ v_new
        s = s * gl_i[..., None, None] + jnp.einsum('bhcd,bhce->bhde', kdec_i, v_new)
        return s, o_i

    xs = tuple(jnp.moveaxis(a, 1, 0) for a in (qg, kdec, u, w, attn, glast))
    s_fin, o = lax.scan(step, s0, xs)
    o = o.transpose(1, 0, 3, 2, 4).reshape(bsz, t, h, -1)
    return o, s_fin


def gdn_bidir(pa, s0_f, s0_b, conv_w, a_log, dt_bias):
    bsz, t, _ = pa.shape
    qkv = jax.nn.silu(dwconv(pa[..., :3 * BRANCH_W], conv_w)).astype(jnp.float32)
    q, k, v = (a.reshape(bsz, t, H_A, HEAD_DIM) for a in jnp.split(qkv, 3, axis=-1))
    q = l2norm(q) * (HEAD_DIM ** -0.5)
    k = l2norm(k)
    ab = pa[..., 3 * BRANCH_W:A_COLS].astype(jnp.float32).reshape(bsz, t, 4, H_A)
    g = -jnp.exp(a_log.astype(jnp.float32)) * jax.nn.softplus(ab[:, :, :2] + dt_bias.astype(jnp.float32))
    beta = jax.nn.sigmoid(ab[:, :, 2:])
    o_f, s_f = gdn_chunked(q, k, v, g[:, :, 0], beta[:, :, 0], s0_f)
    rev = lambda a: jnp.flip(a, axis=1)
    o_b, s_b = gdn_chunked(rev(q), rev(k), rev(v), rev(g[:, :, 1]), rev(beta[:, :, 1]), s0_b)
    return (o_f + rev(o_b)).astype(pa.dtype), s_f, s_b


def spatial_gate(u, v, w_s, b_s):
    bsz, t, _ = v.shape
    n = t // MLP_CHUNK
    vv = v.reshape(bsz, n, MLP_CHUNK, G_D, HEAD_DIM)
    mixed = jnp.einsum('gpq,bnqgd->bnpgd', w_s, vv) + b_s.T[None, None, :, :, None]
    return u * mixed.reshape(bsz, t, BRANCH_W)


def mixer_output(o_a, p, gdn_norm_g, short_conv_w, conf_conv_w, conf_conv_b, conf_ln_g,
                 conf_ln_b, smlp_ln_g, smlp_ln_b, smlp_w, smlp_b, w_out):
    bsz, t, _ = p.shape
    y_a = rmsnorm(o_a, gdn_norm_g).reshape(bsz, t, BRANCH_W)
    off = A_COLS
    b_gate, c_gate, x_in = jnp.split(p[..., off:off + B_COLS], 3, axis=-1)
    y_b = b_gate * dwconv(c_gate * x_in, short_conv_w)
    off += B_COLS
    glu_a, glu_b = jnp.split(p[..., off:off + C_COLS], 2, axis=-1)
    z = dwconv(glu_a * jax.nn.sigmoid(glu_b), conf_conv_w) + conf_conv_b
    y_c = jax.nn.silu(layernorm(z, conf_ln_g, conf_ln_b))
    off += C_COLS
    u, v = jnp.split(p[..., off:off + D_COLS], 2, axis=-1)
    y_d = spatial_gate(u, layernorm(v, smlp_ln_g, smlp_ln_b), smlp_w, smlp_b)
    y = jnp.concatenate([y_a, y_b, y_c, y_d], axis=-1) * jax.nn.silu(p[..., IN_COLS - MIX_W:])
    return y @ w_out


def setup_inputs(seed: int = 0) -> dict:
    key = jax.random.key(seed)
    ks = jax.random.split(key, 24)
    nrm = lambda k, s, sc: jax.random.normal(k, s, jnp.float32) * sc
    dt = jnp.exp(jax.random.uniform(ks[9], (DEPTH, 2, H_A), jnp.float32, np.log(1e-3), np.log(1e-1)))
    return {
        'x': nrm(ks[0], (BATCH, SEQ, D_MODEL), 1.0),
        'c': nrm(ks[1], (BATCH, D_MODEL), 1.0),
        'ctx': nrm(ks[2], (BATCH, CTX_LEN, D_MODEL), 1.0),
        'c_ctx': nrm(ks[3], (D_MODEL,), 1.0),
        'norm_g': 1.0 + nrm(ks[4], (DEPTH, D_MODEL), 0.05),
        'w_ada': nrm(ks[5], (DEPTH, D_MODEL, 3 * D_MODEL), 0.5 * D_MODEL ** -0.5),
        'b_ada': nrm(ks[6], (DEPTH, 3 * D_MODEL), 0.02),
        'w_in': nrm(ks[7], (DEPTH, D_MODEL, IN_COLS), D_MODEL ** -0.5),
        'qkv_conv_w': nrm(ks[8], (DEPTH, QKV_CONV, 3 * BRANCH_W), QKV_CONV ** -0.5),
        'a_log': jnp.log(jax.random.uniform(ks[10], (DEPTH, 2, H_A), jnp.float32, 1.0, 16.0)),
        'dt_bias': dt + jnp.log(-jnp.expm1(-dt)),
        'gdn_norm_g': 1.0 + nrm(ks[11], (DEPTH, HEAD_DIM), 0.05),
        'short_conv_w': nrm(ks[12], (DEPTH, SHORT_CONV, BRANCH_W), SHORT_CONV ** -0.5),
        'conf_conv_w': nrm(ks[13], (DEPTH, CONF_CONV, BRANCH_W), CONF_CONV ** -0.5),
        'conf_conv_b': nrm(ks[14], (DEPTH, BRANCH_W), 0.02),
        'conf_ln_g': 1.0 + nrm(ks[15], (DEPTH, BRANCH_W), 0.05),
        'conf_ln_b': nrm(ks[16], (DEPTH, BRANCH_W), 0.02),
        'smlp_ln_g': 1.0 + nrm(ks[17], (DEPTH, BRANCH_W), 0.05),
        'smlp_ln_b': nrm(ks[18], (DEPTH, BRANCH_W), 0.02),
        'smlp_w': nrm(ks[19], (DEPTH, G_D, MLP_CHUNK, MLP_CHUNK), MLP_CHUNK ** -0.5),
        'smlp_b': 1.0 + nrm(ks[20], (DEPTH, G_D, MLP_CHUNK), 0.02),
        'w_out': nrm(ks[21], (DEPTH, MIX_W, D_MODEL), MIX_W ** -0.5),
        'final_g': 1.0 + nrm(ks[22], (D_MODEL,), 0.05),
    }


def reference(x, c, ctx, c_ctx, norm_g, w_ada, b_ada, w_in, qkv_conv_w, a_log, dt_bias,
              gdn_norm_g, short_conv_w, conf_conv_w, conf_conv_b, conf_ln_g, conf_ln_b,
              smlp_ln_g, smlp_ln_b, smlp_w, smlp_b, w_out, final_g):
    bsz = x.shape[0]
    xc = ctx
    silu_c = jax.nn.silu(c)
    silu_cc = jax.nn.silu(c_ctx)
    s_zero = jnp.zeros((bsz, H_A, HEAD_DIM, HEAD_DIM), jnp.float32)
    for l in range(DEPTH):
        last = l == DEPTH - 1
        mod = silu_c @ w_ada[l] + b_ada[l]
        shift, scale, gate = jnp.split(mod[:, None, :], 3, axis=-1)
        mod_c = silu_cc @ w_ada[l] + b_ada[l]
        shift_c, scale_c, gate_c = jnp.split(mod_c, 3, axis=-1)
        h = rmsnorm(x, norm_g[l]) * (1.0 + scale) + shift
        hc = rmsnorm(xc, norm_g[l]) * (1.0 + scale_c) + shift_c
        col_major = l % 2 == 1
        if col_major:
            h = to_col_major(h)
        p = h @ w_in[l]
        pc = hc @ (w_in[l][:, :A_COLS] if last else w_in[l])
        o_ctx, s_f, s_b = gdn_bidir(pc[..., :A_COLS], s_zero, s_zero, qkv_conv_w[l], a_log[l], dt_bias[l])
        o_lat, _, _ = gdn_bidir(p[..., :A_COLS], s_f, s_b, qkv_conv_w[l], a_log[l], dt_bias[l])
        y = mixer_output(o_lat, p, gdn_norm_g[l], short_conv_w[l], conf_conv_w[l], conf_conv_b[l],
                         conf_ln_g[l], conf_ln_b[l], smlp_ln_g[l], smlp_ln_b[l], smlp_w[l],
                         smlp_b[l], w_out[l])
        if col_major:
            y = from_col_major(y)
        x = x + gate * y
        if not last:
            yc = mixer_output(o_ctx, pc, gdn_norm_g[l], short_conv_w[l], conf_conv_w[l], conf_conv_b[l],
                              conf_ln_g[l], conf_ln_b[l], smlp_ln_g[l], smlp_ln_b[l], smlp_w[l],
                              smlp_b[l], w_out[l])
            xc = xc + gate_c * yc
    return rmsnorm(x, final_g)
```

```python
from contextlib import ExitStack
import numpy as np
import concourse.bass as bass
import concourse.mybir as mybir
from concourse.bass_utils import run_bass_kernel_spmd

F32 = mybir.dt.float32
BF16 = mybir.dt.bfloat16
AF = mybir.ActivationFunctionType
ALU = mybir.AluOpType
AX = mybir.AxisListType

NL = 4
TAPL = 9
STOPL = 0
EPS = 1e-6
NT = 18
WP = 2352
NCH = 29
EPOCH = 4000
NRING = {"sp": 24, "pool": 8, "act": 4}
ENGS = ["pe", "act", "dve", "pool", "sp"]


def wcol(tok):
    return 16 + tok if tok < 256 else tok + 32


class Op:
    __slots__ = ("eng", "fn", "deps", "signal", "sig", "dma", "slot", "dval")


class Sched:
    def __init__(self):
        self.ops = {e: [] for e in ENGS}
        self.lastw = {}
        self.rd = {}

    def add(self, eng, fn, reads=(), writes=(), dma=False):
        nk = lambda k: ("ps", k[1]) if (isinstance(k, tuple) and k[0] == "ps") else k
        reads = list(dict.fromkeys(nk(k) for k in reads))
        writes = list(dict.fromkeys(nk(k) for k in writes))
        op = Op()
        op.eng, op.fn, op.dma, op.signal, op.sig, op.slot, op.dval = eng, fn, dma, False, 0, 0, 0
        deps = []
        for k in reads:
            w = self.lastw.get(k)
            if w is not None:
                deps.append(w)
        for k in writes:
            w = self.lastw.get(k)
            if w is not None:
                deps.append(w)
            r = self.rd.get(k)
            if r:
                deps.extend(r[0].values())
                deps.extend(r[1])
        op.deps = [d for d in dict.fromkeys(deps) if d is not op]
        for d in op.deps:
            if not (d.eng == "pe" and eng == "pe"):
                d.signal = True
        for k in reads:
            r = self.rd.setdefault(k, ({}, []))
            if dma:
                r[1].append(op)
            else:
                r[0][eng] = op
        for k in writes:
            self.lastw[k] = op
            self.rd[k] = ({}, [])
        self.ops[eng].append(op)
        return op

    def emit(self, nc, es):
        nsig = {}
        for e in ENGS:
            cnt = 0
            dcnt = 0
            for op in self.ops[e]:
                if op.dma:
                    op.slot = dcnt % NRING[e]
                    op.dval = 16 * (dcnt // NRING[e] + 1)
                    dcnt += 1
                elif op.signal:
                    cnt += 1
                    op.sig = cnt
            nsig[e] = cnt
        csem = {e: [es.enter_context(nc.semaphore(f"c_{e}_{i}")) for i in range(nsig[e] // EPOCH + 1)]
                for e in ENGS if e != "sp"}
        dsem = {e: [es.enter_context(nc.semaphore(f"d_{e}_{i}")) for i in range(n)] for e, n in NRING.items()}
        block = es.enter_context(nc.Block())

        def run(e, eng):
            wabs = {}
            wd = {}
            for op in self.ops[e]:
                for d in op.deps:
                    if d.dma:
                        key = (d.eng, d.slot)
                        if wd.get(key, 0) >= d.dval:
                            continue
                        eng.wait_ge(dsem[d.eng][d.slot], d.dval)
                        wd[key] = d.dval
                    else:
                        if d.eng == "pe" and e == "pe":
                            continue
                        if wabs.get(d.eng, 0) >= d.sig:
                            continue
                        eng.wait_ge(csem[d.eng][(d.sig - 1) // EPOCH], (d.sig - 1) % EPOCH + 1)
                        wabs[d.eng] = d.sig
                if op.dma and op.dval > 16:
                    key = (e, op.slot)
                    if wd.get(key, 0) < op.dval - 16:
                        eng.wait_ge(dsem[e][op.slot], op.dval - 16)
                        wd[key] = op.dval - 16
                ins = op.fn(eng)
                if op.dma:
                    ins.then_inc(dsem[e][op.slot], 16)
                elif op.signal:
                    ins.then_inc(csem[e][(op.sig - 1) // EPOCH], 1)

        @block.sync
        def _(eng):
            run("sp", eng)

        @block.gpsimd
        def _(eng):
            run("pool", eng)

        @block.scalar
        def _(eng):
            run("act", eng)

        @block.vector
        def _(eng):
            run("dve", eng)

        @block.tensor
        def _(eng):
            run("pe", eng)


class _Stop(Exception):
    pass


def build(nl=NL, nb=2, taps=None, dbg=False, stop=99):
    nc = bass.Bass("TRN2", target_bir_lowering=False)
    es = ExitStack()
    S = Sched()
    dram = lambda name, shape, dt=F32, kind="ExternalInput": nc.dram_tensor(name, list(shape), dt, kind=kind).ap()
    x_d = dram("x", [2, 2048, 1024])
    ctx_d = dram("ctx", [2, 256, 1024])
    ccat_d = dram("ccat", [128, 8, 3])
    consts_d = dram("consts", [128, 9, 128])
    wada_d = dram("w_ada", [NL, 24, 128, 8, 128])
    badaT_d = dram("b_adaT", [NL, 128, 24])
    normgT_d = dram("norm_gT", [NL, 128, 8])
    win_d = dram("w_in", [NL, NCH, 128, 8, 128])
    qkvw_d = dram("qkvw", [NL, 128, 6, 3])
    scw_d = dram("scw", [NL, 128, 2, 3])
    cfw_d = dram("cfw", [NL, 128, 2, 31])
    cvec_d = dram("cvec", [NL, 128, 3, 2])
    rowc_d = dram("rowc", [NL, 128, 784])
    wsT_d = dram("wsT", [NL, 128, 4, 128])
    bsT_d = dram("bsT", [NL, 128, 2, 128])
    wout_d = dram("w_out", [NL, 128, 8, 1024])
    fg_d = dram("final_gB", [128, 1024])
    y_d = dram("y", [2, 2048, 1024], F32, "ExternalOutput")
    xres_d = dram("xres", [2, 2048, 1024], F32, "ExternalOutput" if dbg else "Internal")
    cres_d = dram("cres", [2, 256, 1024], F32, "ExternalOutput" if dbg else "Internal")
    spill_d = dram("spill", [2, 8, 128, 2304], BF16, "Internal")
    tap_d = {}
    if taps:
        for name, shape in taps.items():
            tap_d[name] = dram("tap_" + name, shape, F32, "ExternalOutput")

    sb = lambda name, shape, dt=F32: es.enter_context(nc.sbuf_tensor("s_" + name, list(shape), dt))
    consts = sb("consts", [128, 9, 128])
    IDENT, ONES, TRI, NEGM, SMM, BO = consts[:, 0, :], consts[:, 1, :], (consts[:, 2, :], consts[:, 3, :]), \
        (consts[:, 4, :], consts[:, 5, :]), (consts[:, 6, :], consts[:, 7, :]), consts[:, 8, :]
    identb = sb("identb", [128, 128], BF16)
    wch = sb("wch", [128, 3, 8, 128], BF16)
    wout = sb("wout", [128, 8, 1024], BF16)
    gateB = sb("gateB", [128, 3, 1024])
    scT = sb("scT", [128, 8, 3])
    ccat = sb("ccat", [128, 8, 3])
    modT = sb("modT", [128, 24, 3])
    sc1T = sb("sc1T", [128, 8, 3])
    badaT = sb("badaT", [128, 24])
    normgT = sb("normgT", [128, 8])
    qkvw = sb("qkvw", [128, 6, 3])
    scw = sb("scw", [128, 2, 3])
    cfw = sb("cfw", [128, 2, 31])
    cvec = sb("cvec", [128, 3, 2])
    rowc = sb("rowc", [128, 784])
    negA = sb("negA", [128, 8])
    wsT32 = sb("wsT32", [128, 4, 128])
    wsT = sb("wsT", [128, 4, 128], BF16)
    bsT = sb("bsT", [128, 2, 128])
    fgB = sb("fgB", [128, 1024])
    xt = sb("xt", [128, 2, 1024])
    xn = sb("xn", [128, 2, 1024], BF16)
    junk = sb("junk", [128, 1024], BF16)
    sm = sb("sm", [128, 64])
    qT = sb("qT", [128, 2, WP], BF16)
    kT = sb("kT", [128, 2, WP], BF16)
    kvtok = sb("kvtok", [128, NT, 512], BF16)
    gq = sb("gq", [128, NT, 8])
    bq = sb("bq", [128, NT, 8])
    nbq = sb("nbq", [128, NT, 8])
    s32 = sb("s32", [128, 2, 2, 64])
    s16 = sb("s16", [128, 2, 2, 64], BF16)
    AE = 8 * 2304 + 5 * 2 * WP
    arena = sb("arena", [128, AE], BF16)
    hT = arena[:, 0:8 * 2304].rearrange("p (k n) -> p k n", k=8)
    WS = [arena[:, 8 * 2304 + i * 2 * WP: 8 * 2304 + (i + 1) * 2 * WP].bitcast(F32) for i in range(5)]
    _off = [0]

    def carve(n_el, dt=BF16):
        a = arena[:, _off[0]:_off[0] + n_el * (2 if dt == F32 else 1)]
        _off[0] += n_el * (2 if dt == F32 else 1)
        assert _off[0] <= 8 * 2304
        return a.bitcast(F32) if dt == F32 else a
    Gb = [carve(128, F32) for _ in range(4)]
    decT = [carve(128, F32) for _ in range(4)]
    decTs = [carve(128, F32) for _ in range(4)]
    Egc = [carve(128, F32) for _ in range(4)]
    XM = [[carve(384) for _ in range(2)] for _ in range(4)]
    ATb = [carve(128) for _ in range(4)]
    negwT = [carve(128) for _ in range(4)]
    qgT = [carve(128) for _ in range(4)]
    kgb = [carve(128) for _ in range(4)]
    kdec = [carve(128) for _ in range(4)]
    kTz = [carve(128) for _ in range(4)]
    tmpS = [carve(64, F32) for _ in range(4)]
    otmp = [carve(64, F32) for _ in range(4)]
    vnew = [[carve(64) for _ in range(2)] for _ in range(4)]
    tsm = carve(32, F32)
    osum = carve(256, F32)
    osq = carve(256, F32)
    yab = carve(256)
    yTt = carve(8 * 128).rearrange("p (k n) -> p k n", k=8)
    gat = carve(2 * 128).rearrange("p (k n) -> p k n", k=2)
    rt = carve(1024, F32)
    of_ap = arena[:, 8 * 2304: 8 * 2304 + 2 * NT * 256].bitcast(F32).rearrange("p (t n) -> p t n", t=NT)
    wa = arena[:, 8 * 2304 + 4 * WP: 8 * 2304 + 4 * WP + 2 * 2 * 1024].bitcast(F32).rearrange("p (i k n) -> p i k n", i=2, k=8)
    Dg = WS[4][:, 0:512]
    ps = [es.enter_context(nc.psum_tensor(f"ps{i}", [128, 512], F32)) for i in range(8)]

    def pk(b, r0=0, r1=4):
        return [("ps", b, r) for r in range(r0, r1)]

    ARK = ["arenaH"]
    WSK = lambda i: [("WS", i)]
    HK = lambda t0, t1: [("hT", t) for t in range(t0, t1)]

    S.add("sp", lambda e: e.dma_start(out=consts[:], in_=consts_d[:]), [], ["consts"], dma=True)
    S.add("sp", lambda e: e.dma_start(out=ccat[:], in_=ccat_d[:]), [], ["ccat"], dma=True)
    S.add("sp", lambda e: e.dma_start(out=fgB[:], in_=fg_d[:]), [], ["fgB"], dma=True)
    S.add("dve", lambda e: e.tensor_copy(out=identb[:], in_=IDENT), ["consts"], ["identb"])
    S.add("act", lambda e: e.activation(out=scT[:], in_=ccat[:], func=AF.Silu), ["ccat"], ["scT"])
    for i in range(5):
        S.add("pool", lambda e, i=i: e.memset(WS[i], 0.0), [], WSK(i))

    evac_rr = [0]
    out_keys = []
    bar_n = [0]
    bscr = sb("bscr", [128, 16])
    bdram = dram("bdram", [2, 128, 4], F32, "Internal")

    def full_barrier(extra_writes=()):
        n = bar_n[0]
        bar_n[0] += 1
        S.add("pe", lambda e: e.matmul(ps[0][:, 0:128], identb[:], identb[:], start=True, stop=True), ["identb"], [("bar1", n, "pe")] + pk(0))
        S.add("act", lambda e: e.activation(out=bscr[:, 0:1], in_=bscr[:, 8:9], func=AF.Copy), ["bscr8"], [("bar1", n, "act"), "bscr0"])
        S.add("dve", lambda e: e.tensor_copy(out=bscr[:, 1:2], in_=bscr[:, 8:9]), ["bscr8"], [("bar1", n, "dve"), "bscr1"])
        S.add("sp", lambda e: e.dma_start(out=bdram[0], in_=bscr[:, 8:12]), ["bscr8"], [("bar1", n, "sp"), "bdram0"], dma=True)
        S.add("pool", lambda e: e.memset(bscr[:, 2:3], 0.0), [("bar1", n, k) for k in ("pe", "act", "dve", "sp")],
              [("bar2", n), "bscr2"] + list(extra_writes))
        S.add("pe", lambda e: e.matmul(ps[0][:, 0:128], identb[:], identb[:], start=True, stop=True), [("bar2", n), "identb"], pk(0))
        S.add("act", lambda e: e.activation(out=bscr[:, 3:4], in_=bscr[:, 8:9], func=AF.Copy), [("bar2", n), "bscr8"], ["bscr3"])
        S.add("dve", lambda e: e.tensor_copy(out=bscr[:, 4:5], in_=bscr[:, 8:9]), [("bar2", n), "bscr8"], ["bscr4"])
        S.add("sp", lambda e: e.dma_start(out=bdram[1], in_=bscr[:, 8:12]), [("bar2", n), "bscr8"], ["bdram1"], dma=True)

    S.add("pool", lambda e: e.memset(bscr[:], 0.0), [], ["bscr8", "bscr0", "bscr1", "bscr2", "bscr3", "bscr4"])

    def evac_copy(dst, src, reads, writes, scale=None):
        evac_rr[0] ^= 1
        if evac_rr[0]:
            S.add("act", lambda e: e.activation(out=dst, in_=src, func=AF.Copy, scale=(1.0 if scale is None else scale)),
                  reads, writes)
        else:
            if scale is None:
                S.add("dve", lambda e: e.tensor_copy(out=dst, in_=src), reads, writes)
            else:
                S.add("dve", lambda e: e.tensor_scalar(out=dst, in0=src, scalar1=float(scale), scalar2=None, op0=ALU.mult),
                      reads, writes)

    def tap(name, ap, reads):
        if name in tap_d:
            S.add("pool", lambda e: e.dma_start(out=tap_d[name][:], in_=ap), reads, [("tap", name)], dma=True)

    wch_i = [0]
    psrot = [0]

    def load_wch(l, cc):
        i = wch_i[0] % 3
        wch_i[0] += 1
        S.add("pool", lambda e: e.dma_start(out=wch[:, i, :, :], in_=win_d[l, cc]), [], [("wch", i)], dma=True)
        return i

    GROUPS = [(0, 256)] + [(256 + 512 * g, 512) for g in range(4)]

    def proj_fm(l, cc, dst, dkeys, func=None):
        i = load_wch(l, cc)
        for (t0, n) in GROUPS:
            bnk = psrot[0] % 4
            psrot[0] += 1
            for kc in range(8):
                S.add("pe", lambda e, kc=kc, bnk=bnk, t0=t0, n=n: e.matmul(
                    ps[bnk][:, 0:n], wch[:, i, kc, :], hT[:, kc, t0:t0 + n], start=(kc == 0), stop=(kc == 7)),
                    [("wch", i)] + HK(t0 // 128, (t0 + n) // 128), pk(bnk))
            d = dst[:, wcol(t0):wcol(t0) + n]
            if func is None:
                evac_copy(d, ps[bnk][:, 0:n], pk(bnk), dkeys)
            else:
                S.add("act", lambda e, d=d, bnk=bnk, n=n: e.activation(out=d, in_=ps[bnk][:, 0:n], func=func),
                      pk(bnk), dkeys)

    R0, R1 = 16, 2336

    def zero_mid(i):
        S.add("pool", lambda e: e.memset(WS[i][:, 272:288], 0.0), [], WSK(i))

    def spill(b, c, i):
        for (c0, n0, n) in ((16, 0, 256), (288, 256, 1024), (1312, 1280, 1024)):
            S.add("pool", lambda e, c0=c0, n0=n0, n=n: e.dma_start(out=spill_d[b, c, :, n0:n0 + n], in_=WS[i][:, c0:c0 + n]),
                  WSK(i), [("spill", b, c, n0)], dma=True)

    def tt(eng, out, a, b_, op, reads, writes):
        S.add(eng, lambda e: e.tensor_tensor(out=out, in0=a, in1=b_, op=op), reads, writes)

    def conv_taps(dst, src, wts, ntap, bias, reads, writes):
        half = ntap // 2
        if bias is None:
            S.add("dve", lambda e: e.tensor_scalar(out=dst[:, R0:R1], in0=src[:, R0 - half:R1 - half], scalar1=wts[:, 0:1],
                                                   scalar2=None, op0=ALU.mult), reads, writes)
        else:
            S.add("dve", lambda e: e.tensor_scalar(out=dst[:, R0:R1], in0=src[:, R0 - half:R1 - half], scalar1=wts[:, 0:1],
                                                   scalar2=bias, op0=ALU.mult, op1=ALU.add), reads, writes)
        for k in range(1, ntap):
            S.add("dve", lambda e, k=k: e.scalar_tensor_tensor(out=dst[:, R0:R1], in0=src[:, R0 + k - half:R1 + k - half],
                                                               scalar=wts[:, k:k + 1], in1=dst[:, R0:R1], op0=ALU.mult, op1=ALU.add),
                  reads + writes, writes)

    def rsqrt_inplace(ap, scale, reads_writes):
        S.add("act", lambda e: e.activation(out=ap, in_=ap, func=AF.Sqrt, bias=epsc[:, 0:1], scale=scale), reads_writes + ["epsc"], reads_writes)
        S.add("dve", lambda e: e.reciprocal(out=ap, in_=ap), reads_writes, reads_writes)

    epsc = sb("epsc", [128, 2])
    S.add("pool", lambda e: e.memset(epsc[:, 0:1], EPS), [], ["epsc"])
    S.add("pool", lambda e: e.memset(epsc[:, 1:2], 1.0), ["epsc"], ["epsc"])

    def xsrc(l, b, t):
        if t < 2:
            base = ctx_d if l == 0 else cres_d
            return [(base[b, t * 128:(t + 1) * 128, :], slice(0, 128))]
        base = x_d if l == 0 else xres_d
        tt_ = t - 2
        if l % 2 == 0:
            return [(base[b, tt_ * 128:(tt_ + 1) * 128, :], slice(0, 128))]
        v = base[b].rearrange("(r c) d -> c r d", c=64)
        return [(v[4 * tt_ + i], slice(32 * i, 32 * i + 32)) for i in range(4)]

    def xdst(l, b, t, last):
        if t < 2:
            return [(cres_d[b, t * 128:(t + 1) * 128, :], slice(0, 128))]
        base = y_d if last else xres_d
        tt_ = t - 2
        if l % 2 == 0:
            return [(base[b, tt_ * 128:(tt_ + 1) * 128, :], slice(0, 128))]
        v = base[b].rearrange("(r c) d -> c r d", c=64)
        return [(v[4 * tt_ + i], slice(32 * i, 32 * i + 32)) for i in range(4)]

    def xkeys(b, t):
        return [("cr", b, t)] if t < 2 else [("xr", b, tt_, i_) for tt_ in range(16) for i_ in range(4)]

    def XTK(slot):
        return [("xt", slot, i_) for i_ in range(4)]

    def load_xt(l, b, t, slot):
        srcs = xsrc(l, b, t)
        for i_, (src, psl) in enumerate(srcs):
            S.add("sp", lambda e, src=src, psl=psl: e.dma_start(out=xt[psl, slot, :], in_=src),
                  (xkeys(b, t) if l > 0 else []), (XTK(slot) if len(srcs) == 1 else [("xt", slot, i_)]), dma=True)

    cur_l = [0]

    def chk(level):
        if stop == level and cur_l[0] == STOPL:
            raise _Stop()

    def layer_prologue(l, last):
        chk(10)
        for dst, src, key in ((badaT, badaT_d[l], "badaT"), (normgT, normgT_d[l], "normgT"), (qkvw, qkvw_d[l], "qkvw"),
                              (scw, scw_d[l], "scw"), (cfw, cfw_d[l], "cfw"), (cvec, cvec_d[l], "cvec"),
                              (rowc, rowc_d[l], "rowc"), (wsT32, wsT_d[l], "wsT32"), (bsT, bsT_d[l], "bsT")):
            S.add("sp", lambda e, dst=dst, src=src: e.dma_start(out=dst[:], in_=src), [], [key], dma=True)
        for h2 in range(8):
            S.add("pool", lambda e, h2=h2: e.dma_start(out=wout[:, h2, :], in_=wout_d[l, :, h2, :]),
                  [], [("wout", h2)], dma=True)
        S.add("dve", lambda e: e.tensor_copy(out=wsT[:], in_=wsT32[:]), ["wsT32"], ["wsT"])
        S.add("act", lambda e: e.activation(out=negA[:], in_=rowc[:, 768:776], func=AF.Exp), ["rowc"], ["negA"])
        S.add("dve", lambda e: e.tensor_scalar(out=negA[:], in0=negA[:], scalar1=-1.0, scalar2=None, op0=ALU.mult), ["negA"], ["negA"])
        chk(11)
        for cc in range(24):
            wi = cc % 2
            S.add("sp", lambda e, cc=cc, wi=wi: e.dma_start(out=wa[:, wi, :, :], in_=wada_d[l, cc]),
                  [], [("wa", wi)] + WSK(2) + WSK(3), dma=True)
            reg = 6 + cc % 2
            for kc in range(8):
                S.add("pe", lambda e, kc=kc, wi=wi, reg=reg: e.matmul(ps[reg][:, 0:3], wa[:, wi, kc, :], scT[:, kc, :],
                                                                    start=(kc == 0), stop=(kc == 7)),
                      [("wa", wi), "scT"] + WSK(2) + WSK(3), [("ps", reg, 0)])
            S.add("dve", lambda e, cc=cc, reg=reg: e.tensor_scalar(out=modT[:, cc, :], in0=ps[reg][:, 0:3],
                                                                   scalar1=badaT[:, cc:cc + 1], scalar2=None, op0=ALU.add),
                  [("ps", reg, 0), "badaT"], [("modT", cc)])
        for kc in range(8):
            S.add("dve", lambda e, kc=kc: e.tensor_scalar(out=sc1T[:, kc, :], in0=modT[:, 8 + kc, :], scalar1=1.0,
                                                          scalar2=normgT[:, kc:kc + 1], op0=ALU.add, op1=ALU.mult),
                  [("modT", 8 + kc), "normgT"], [("sc1T", kc)])
        for r in range(3):
            for hf in range(2):
                for j in range(4):
                    kc = hf * 4 + j
                    S.add("dve", lambda e, j=j, kc=kc, r=r: e.tensor_scalar(out=Dg[:, j * 128:(j + 1) * 128], in0=IDENT,
                                                                           scalar1=modT[:, 16 + kc, r:r + 1], scalar2=None, op0=ALU.mult),
                          [("modT", 16 + kc), "consts"], WSK(4))
                bnk = 4 + (r * 2 + hf) % 2
                S.add("pe", lambda e, bnk=bnk: e.matmul(ps[bnk][:, :], ONES, Dg, start=True, stop=True), WSK(4) + ["consts"], pk(bnk))
                evac_copy(gateB[:, r, hf * 512:(hf + 1) * 512], ps[bnk][:, :], pk(bnk), [("gateB", r)])

        if l == TAPL:
            tap("modT", modT[:].rearrange("p a b -> p (a b)"), [("modT", i_) for i_ in range(24)])
            tap("sc1T", sc1T[:].rearrange("p a b -> p (a b)"), [("sc1T", i_) for i_ in range(8)])
            tap("gateB", gateB[:, 2, :], [("gateB", 2)])

    def batch_body(l, b, last):
        for t in range(NT):
            slot = t % 2
            r = 2 if t < 2 else b
            load_xt(l, b, t, slot)
            xk = XTK(slot)
            if l == TAPL and b == 0 and t == 0:
                tap("xt0", xt[:, 0, :], xk)
            if l == TAPL and b == 0 and t == 2:
                tap("xt2", xt[:, 0, :], xk)
            S.add("act", lambda e, slot=slot: e.activation(out=junk[:], in_=xt[:, slot, :], func=AF.Square, accum_out=sm[:, slot:slot + 1]),
                  xk, ["junk", ("sm", slot)])
            rsqrt_inplace(sm[:, slot:slot + 1], 1.0 / 1024, [("sm", slot)])
            S.add("act", lambda e, slot=slot: e.activation(out=xn[:, slot, :], in_=xt[:, slot, :], func=AF.Copy, scale=sm[:, slot:slot + 1]),
                  xk + [("sm", slot)], [("xn", slot)])
            for hf in range(2):
                bnk = 4 + hf
                for j in range(4):
                    kc = hf * 4 + j
                    S.add("pe", lambda e, j=j, kc=kc, bnk=bnk, slot=slot: e.matmul(
                        ps[bnk][:, j * 128:(j + 1) * 128], xn[:, slot, kc * 128:(kc + 1) * 128], identb[:], start=True, stop=True),
                        [("xn", slot), "identb"], [("ps", bnk, j)])
                for j in range(4):
                    kc = hf * 4 + j
                    o_ = hT[:, kc, t * 128:(t + 1) * 128]
                    i_ = ps[bnk][:, j * 128:(j + 1) * 128]
                    rk = [("ps", bnk, j), ("sc1T", kc), ("modT", kc)]
                    if j % 2 == 0:
                        S.add("act", lambda e, o_=o_, i_=i_, kc=kc, r=r: e.activation(out=o_, in_=i_, func=AF.Identity,
                                                                                  scale=sc1T[:, kc, r:r + 1], bias=modT[:, kc, r:r + 1]),
                              rk, [("hT", t)])
                    else:
                        S.add("dve", lambda e, o_=o_, i_=i_, kc=kc, r=r: e.tensor_scalar(out=o_, in0=i_, scalar1=sc1T[:, kc, r:r + 1],
                                                                                      scalar2=modT[:, kc, r:r + 1], op0=ALU.mult, op1=ALU.add),
                              rk, [("hT", t)])

        if l == TAPL and b == 0:
            for i_ in range(2):
                tap(f"hT{i_}", hT[:, 0, i_ * 1152:(i_ + 1) * 1152], HK(0, NT))
        chk(2)
        for i_ in range(5):
            for (a_, b_) in ((0, 16), (272, 288), (2336, 2352)):
                S.add("pool", lambda e, i_=i_, a_=a_, b_=b_: e.memset(WS[i_][:, a_:b_], 0.0), [], WSK(i_))
        for j in range(2):
            proj_fm(l, 21 + j, WS[j], WSK(j), AF.Silu)
            spill(b, j, j)
        for j in range(2):
            zero_mid(0), zero_mid(1)
            proj_fm(l, 9 + j, WS[0], WSK(0))
            proj_fm(l, 11 + j, WS[1], WSK(1))
            tt("pool", WS[0][:, R0:R1], WS[0][:, R0:R1], WS[1][:, R0:R1], ALU.mult, WSK(0) + WSK(1), WSK(0))
            conv_taps(WS[1], WS[0], scw[:, j, :], 3, None, WSK(0) + ["scw"], WSK(1))
            proj_fm(l, 7 + j, WS[2], WSK(2))
            proj_fm(l, 23 + j, WS[3], WSK(3), AF.Silu)
            tt("pool", WS[2][:, R0:R1], WS[2][:, R0:R1], WS[1][:, R0:R1], ALU.mult, WSK(2) + WSK(1), WSK(2))
            tt("dve", WS[2][:, R0:R1], WS[2][:, R0:R1], WS[3][:, R0:R1], ALU.mult, WSK(2) + WSK(3), WSK(2))
            spill(b, 2 + j, 2)
        for j in range(2):
            zero_mid(0)
            proj_fm(l, 13 + j, WS[0], WSK(0))
            proj_fm(l, 15 + j, WS[1], WSK(1), AF.Sigmoid)
            tt("pool", WS[0][:, R0:R1], WS[0][:, R0:R1], WS[1][:, R0:R1], ALU.mult, WSK(0) + WSK(1), WSK(0))
            conv_taps(WS[2 + j], WS[0], cfw[:, j, :], 31, cvec[:, 0, j:j + 1], WSK(0) + ["cfw", "cvec"], WSK(2 + j))
        for (t0, n) in GROUPS:
            c0 = wcol(t0)
            sq = [WS[0][:, 0:512], WS[0][:, 512:1024]]
            mg, vg = WS[0][:, 1024:1536], WS[0][:, 1536:2048]
            for j in range(2):
                S.add("act", lambda e, j=j, c0=c0, n=n: e.activation(out=sq[j][:, 0:n], in_=WS[2 + j][:, c0:c0 + n], func=AF.Square),
                      WSK(2 + j), WSK(0))
            for j in range(2):
                S.add("pe", lambda e, j=j, c0=c0, n=n: e.matmul(ps[4][:, 0:n], ONES, WS[2 + j][:, c0:c0 + n], start=(j == 0), stop=(j == 1)),
                      WSK(2 + j) + ["consts"], pk(4))
            for j in range(2):
                S.add("pe", lambda e, j=j, n=n: e.matmul(ps[5][:, 0:n], ONES, sq[j][:, 0:n], start=(j == 0), stop=(j == 1)),
                      WSK(0) + ["consts"], pk(5))
            S.add("act", lambda e, n=n: e.activation(out=mg[:, 0:n], in_=ps[4][:, 0:n], func=AF.Copy, scale=1.0 / 256), pk(4), WSK(0))
            S.add("act", lambda e, n=n: e.activation(out=vg[:, 0:n], in_=ps[4][:, 0:n], func=AF.Square, scale=1.0 / 256), pk(4), WSK(0))
            S.add("dve", lambda e, n=n: e.scalar_tensor_tensor(out=vg[:, 0:n], in0=ps[5][:, 0:n], scalar=1.0 / 256, in1=vg[:, 0:n],
                                                              op0=ALU.mult, op1=ALU.subtract), pk(5) + WSK(0), WSK(0))
            rsqrt_inplace(vg[:, 0:n], 1.0, WSK(0))
            for j in range(2):
                z = WS[2 + j][:, c0:c0 + n]
                tt("dve", z, z, mg[:, 0:n], ALU.subtract, WSK(2 + j) + WSK(0), WSK(2 + j))
                tt("pool", z, z, vg[:, 0:n], ALU.mult, WSK(2 + j) + WSK(0), WSK(2 + j))
        for j in range(2):
            z = WS[2 + j][:, R0:R1]
            S.add("act", lambda e, z=z, j=j: e.activation(out=z, in_=z, func=AF.Silu, scale=cvec[:, 1, j:j + 1], bias=cvec[:, 2, j:j + 1]),
                  WSK(2 + j) + ["cvec"], WSK(2 + j))
            proj_fm(l, 25 + j, WS[1], WSK(1), AF.Silu)
            tt("dve", z, z, WS[1][:, R0:R1], ALU.mult, WSK(2 + j) + WSK(1), WSK(2 + j))
            spill(b, 4 + j, 2 + j)
        wiA = load_wch(l, 19)
        wiB = load_wch(l, 20)
        vt32 = WS[0][:, 0:256]
        vnz = WS[1][:, 0:256].bitcast(BF16)
        vnz3 = vnz.rearrange("p (j g x) -> p j g x", j=2, g=2)
        vt3 = vt32.rearrange("p (j g d) -> p j g d", j=2, g=2)
        lnb3 = rowc[:, 256:512].rearrange("p (j g d) -> p j g d", j=2, g=2)
        S.add("pool", lambda e: e.memset(WS[1][:, 0:256], 0.0), [], WSK(1))
        for t in range(NT):
            for jj, wi in ((0, wiA), (1, wiB)):
                for kc in range(8):
                    S.add("pe", lambda e, jj=jj, wi=wi, kc=kc, t=t: e.matmul(ps[4][:, jj * 128:(jj + 1) * 128], hT[:, kc, t * 128:(t + 1) * 128],
                                                                            wch[:, wi, kc, :], start=(kc == 0), stop=(kc == 7)),
                          [("wch", wi), ("hT", t)], pk(4, jj, jj + 1))
            S.add("act", lambda e: e.activation(out=vt32, in_=ps[4][:, 0:256], func=AF.Copy, accum_out=sm[:, 8:9]), pk(4, 0, 2), WSK(0) + [("sm", 8)])
            S.add("act", lambda e: e.activation(out=junk[:, 0:256], in_=ps[4][:, 0:256], func=AF.Square, accum_out=sm[:, 9:10]), pk(4, 0, 2), ["junk", ("sm", 9)])
            S.add("dve", lambda e: e.tensor_scalar(out=sm[:, 8:9], in0=sm[:, 8:9], scalar1=1.0 / 256, scalar2=None, op0=ALU.mult), [("sm", 8)], [("sm", 8)])
            S.add("dve", lambda e: e.tensor_tensor(out=sm[:, 10:11], in0=sm[:, 8:9], in1=sm[:, 8:9], op=ALU.mult), [("sm", 8)], [("sm", 10)])
            S.add("dve", lambda e: e.scalar_tensor_tensor(out=sm[:, 9:10], in0=sm[:, 9:10], scalar=1.0 / 256, in1=sm[:, 10:11], op0=ALU.mult, op1=ALU.subtract),
                  [("sm", 9), ("sm", 10)], [("sm", 9)])
            rsqrt_inplace(sm[:, 9:10], 1.0, [("sm", 9)])
            S.add("dve", lambda e: e.tensor_scalar(out=vt32, in0=vt32, scalar1=sm[:, 8:9], scalar2=sm[:, 9:10], op0=ALU.subtract, op1=ALU.mult),
                  WSK(0) + [("sm", 8), ("sm", 9)], WSK(0))
            tt("pool", vt32, vt32, rowc[:, 0:256], ALU.mult, WSK(0) + ["rowc"], WSK(0))
            for g2 in range(2):
                tt("dve", vnz3[:, :, g2, g2 * 64:(g2 + 1) * 64], vt3[:, :, g2, :], lnb3[:, :, g2, :], ALU.add, WSK(0) + ["rowc"], WSK(1))
            for j in range(2):
                for g2 in range(2):
                    g = 2 * j + g2
                    S.add("pe", lambda e, j=j, g2=g2, g=g: e.matmul(ps[5][:, j * 128:(j + 1) * 128], vnz3[:, j, g2, :],
                                                                   wsT[:, g, :], start=(g2 == 0), stop=(g2 == 1)),
                          WSK(1) + ["wsT"], pk(5, j, j + 1))
            for j in range(2):
                c0 = wcol(t * 128)
                tt("dve", WS[2 + j][:, c0:c0 + 128], ps[5][:, j * 128:(j + 1) * 128], bsT[:, j, :], ALU.add, pk(5, j, j + 1) + ["bsT"], WSK(2 + j))
        for j in range(2):
            proj_fm(l, 17 + j, WS[0], WSK(0))
            proj_fm(l, 27 + j, WS[1], WSK(1), AF.Silu)
            tt("pool", WS[0][:, R0:R1], WS[0][:, R0:R1], WS[2 + j][:, R0:R1], ALU.mult, WSK(0) + WSK(2 + j), WSK(0))
            tt("dve", WS[0][:, R0:R1], WS[0][:, R0:R1], WS[1][:, R0:R1], ALU.mult, WSK(0) + WSK(1), WSK(0))
            spill(b, 6 + j, 0)

        chk(3)
        for cc in range(6):
            src = WS[cc % 2]
            zero_mid(cc % 2)
            proj_fm(l, cc, src, WSK(cc % 2))
            conv_taps(WS[2], src, qkvw[:, cc, :], 3, None, WSK(cc % 2) + ["qkvw"], WSK(2))
            S.add("act", lambda e: e.activation(out=WS[2][:, R0:R1], in_=WS[2][:, R0:R1], func=AF.Silu), WSK(2), WSK(2))
            if cc < 4:
                dstT = qT if cc < 2 else kT
                S.add("act", lambda e: e.activation(out=WS[3][:, R0:R1], in_=WS[2][:, R0:R1], func=AF.Square), WSK(2), WSK(3))
                for (t0, n) in GROUPS:
                    c0 = wcol(t0)
                    S.add("pe", lambda e, c0=c0, n=n: e.matmul(ps[4][:, 0:n], BO, WS[3][:, c0:c0 + n], start=True, stop=True),
                          WSK(3) + ["consts"], pk(4))
                    rn = WS[4][:, 0:n]
                    S.add("act", lambda e, n=n, rn=rn: e.activation(out=rn, in_=ps[4][:, 0:n], func=AF.Sqrt, bias=epsc[:, 0:1], scale=1.0),
                          pk(4) + ["epsc"], WSK(4))
                    S.add("dve", lambda e, rn=rn: e.reciprocal(out=rn, in_=rn), WSK(4), WSK(4))
                    S.add("dve", lambda e, c0=c0, n=n, rn=rn, dstT=dstT, cc=cc: e.scalar_tensor_tensor(
                        out=dstT[:, cc % 2, c0:c0 + n], in0=WS[2][:, c0:c0 + n], scalar=(0.125 if cc < 2 else 1.0), in1=rn,
                        op0=ALU.mult, op1=ALU.mult), WSK(2) + WSK(4), [("qkT", cc)])
            if cc >= 2:
                for t in range(NT):
                    c0 = wcol(t * 128)
                    bnk = 5 + t % 2
                    if cc < 4:
                        S.add("pe", lambda e, c0=c0, bnk=bnk, cc=cc: e.matmul(ps[bnk][:, 0:128], kT[:, cc % 2, c0:c0 + 128], identb[:], start=True, stop=True),
                              [("qkT", cc), "identb"], pk(bnk, 0, 1))
                    else:
                        S.add("pe", lambda e, c0=c0, bnk=bnk: e.matmul(ps[bnk][:, 0:128], WS[2][:, c0:c0 + 128], IDENT, start=True, stop=True),
                              WSK(2) + ["consts"], pk(bnk, 0, 1))
                    evac_copy(kvtok[:, t, (cc - 2) * 128:(cc - 1) * 128], ps[bnk][:, 0:128], pk(bnk, 0, 1), [("kvtok", t)])
        wi = load_wch(l, 6)
        for t in range(NT):
            bnk = 5 + t % 2
            for kc in range(8):
                S.add("pe", lambda e, kc=kc, t=t, bnk=bnk: e.matmul(ps[bnk][:, 128:144], hT[:, kc, t * 128:(t + 1) * 128], wch[:, wi, kc, 0:16],
                                                                   start=(kc == 0), stop=(kc == 7)),
                      [("wch", wi), ("hT", t)], pk(bnk, 1, 2))
            tmp = sm[:, 16:24]
            tt("dve", tmp, ps[bnk][:, 128:136], rowc[:, 776:784], ALU.add, pk(bnk, 1, 2) + ["rowc"], [("sm", 16)])
            S.add("act", lambda e, tmp=tmp: e.activation(out=tmp, in_=tmp, func=AF.Exp), [("sm", 16)], [("sm", 16)])
            S.add("act", lambda e, tmp=tmp: e.activation(out=tmp, in_=tmp, func=AF.Ln, bias=epsc[:, 1:2], scale=1.0), [("sm", 16), "epsc"], [("sm", 16)])
            tt("dve", gq[:, t, :], tmp, negA[:], ALU.mult, [("sm", 16), "negA"], [("gb", t)])
            S.add("act", lambda e, t=t, bnk=bnk: e.activation(out=bq[:, t, :], in_=ps[bnk][:, 136:144], func=AF.Sigmoid), pk(bnk, 1, 2), [("gb", t)])
            S.add("dve", lambda e, t=t: e.tensor_scalar(out=nbq[:, t, :], in0=bq[:, t, :], scalar1=-1.0, scalar2=None, op0=ALU.mult), [("gb", t)], [("gb", t)])

        chk(4)
        full_barrier()
        for h_ in range(4):
            S.add("pool", lambda e, h_=h_: e.memset(negwT[h_], 0.0), [], [("negwT", h_)])
            S.add("pool", lambda e, h_=h_: e.memset(qgT[h_], 0.0), [], [("qgT", h_)])
            S.add("pool", lambda e, h_=h_: e.memset(kgb[h_], 0.0), [], [("kg", h_)])
            S.add("pool", lambda e, h_=h_: e.memset(kdec[h_], 0.0), [], [("kdec", h_)])
            S.add("pool", lambda e, h_=h_: e.memset(kTz[h_], 0.0), [], [("kTz", h_)])
            for c_ in range(2):
                S.add("pool", lambda e, h_=h_, c_=c_: e.memset(vnew[h_][c_], 0.0), [], [("vnew", h_, c_)])
        chk(5)
        S.add("pool", lambda e: e.memset(s32[:], 0.0), [], [("s", d, h) for d in range(2) for h in range(4)])
        S.add("pool", lambda e: e.memset(s16[:], 0.0), [], [("s16", d, h) for d in range(2) for h in range(4)])
        allH = HK(0, NT)

        def prep_tile(t, d):
            c0 = wcol(t * 128)
            pg = ps[1][:, 384:392]
            S.add("pe", lambda e: e.matmul(ps[1][:, 384:388], TRI[d], gq[:, t, 4 * d:4 * d + 4], start=True, stop=True), [("gb", t), "consts"], [("ps", 1, 3)])
            S.add("pe", lambda e: e.matmul(ps[1][:, 388:392], BO, gq[:, t, 4 * d:4 * d + 4], start=True, stop=True), [("gb", t), "consts"], [("ps", 1, 3)])
            TK = ["tsm"]
            S.add("dve", lambda e: e.tensor_scalar(out=tsm[:, 0:4], in0=ps[1][:, 384:388], scalar1=-1.0, scalar2=None, op0=ALU.mult),
                  [("ps", 1, 3)], TK)
            S.add("act", lambda e: e.activation(out=tsm[:, 4:8], in_=ps[1][:, 384:388], func=AF.Exp), [("ps", 1, 3)], TK)
            tt("dve", tsm[:, 12:16], ps[1][:, 388:392], tsm[:, 0:4], ALU.add, [("ps", 1, 3)] + TK, TK)
            S.add("act", lambda e: e.activation(out=tsm[:, 8:12], in_=tsm[:, 12:16], func=AF.Exp), TK, TK)
            for h in range(4):
                pb, hp, col = 64 * (h % 2), h // 2, 4 * d + h
                A, Bk = ps[2 * h], ps[2 * h + 1]
                kTh = kT[pb:pb + 64, hp, c0:c0 + 128]
                qTh = qT[pb:pb + 64, hp, c0:c0 + 128]
                hk = [("H", h)]
                S.add("pool", lambda e, h=h, pb=pb, kTh=kTh: e.tensor_copy(out=kTz[h][pb:pb + 64, :], in_=kTh), [("qkT", 2), ("qkT", 3)], [("kTz", h)])
                kTf = kT[:, hp, c0:c0 + 128]
                qTf = qT[:, hp, c0:c0 + 128]
                S.add("pe", lambda e, A=A, h=h, kTf=kTf: e.matmul(A[:, 0:128], kTz[h], kTf, start=True, stop=True), [("kTz", h), ("qkT", 2), ("qkT", 3)], [("ps", 2 * h, 0)])
                S.add("pe", lambda e, A=A, h=h, qTf=qTf: e.matmul(A[:, 128:256], kTz[h], qTf, start=True, stop=True),
                      [("kTz", h), ("qkT", 0), ("qkT", 1)], [("ps", 2 * h, 1)])
                S.add("pool", lambda e, h=h, col=col: e.tensor_scalar(out=Gb[h], in0=TRI[d], scalar1=gq[:, t, col:col + 1], scalar2=0.0, op0=ALU.mult, op1=ALU.add),
                      [("gb", t), "consts"], [("G", h)])
                S.add("pe", lambda e, A=A, h=h: e.matmul(A[:, 256:384], ONES, Gb[h], start=True, stop=False), [("G", h), "consts"], [("ps", 2 * h, 2)])
                S.add("pe", lambda e, A=A: e.matmul(A[:, 256:384], IDENT, NEGM[d], start=False, stop=True), ["consts"], [("ps", 2 * h, 2)])
                S.add("pe", lambda e, A=A, h=h: e.matmul(A[:, 384:512], ONES, Gb[h], start=True, stop=True), [("G", h), "consts"], [("ps", 2 * h, 3)])
                S.add("act", lambda e, A=A, h=h: e.activation(out=decT[h], in_=A[:, 256:384], func=AF.Exp, bias=tsm[:, h:h + 1], scale=1.0),
                      [("ps", 2 * h, 2)] + TK, [("decT", h)])
                S.add("act", lambda e, A=A, h=h: e.activation(out=Egc[h], in_=A[:, 384:512], func=AF.Exp), [("ps", 2 * h, 3)], [("Egc", h)])
                tt("pool", decTs[h], decT[h], SMM[d], ALU.mult, [("decT", h), "consts"], [("decTs", h)])
                X0 = XM[h][0]
                S.add("dve", lambda e, A=A, X0=X0, h=h, col=col: e.scalar_tensor_tensor(out=X0[:, 128:256], in0=A[:, 0:128], scalar=nbq[:, t, col:col + 1],
                                                                                   in1=decTs[h], op0=ALU.mult, op1=ALU.mult),
                      [("ps", 2 * h, 0), ("gb", t), ("decTs", h)], [("XM", h, 0)])
                tt("dve", ATb[h], A[:, 128:256], decT[h], ALU.mult, [("ps", 2 * h, 1), ("decT", h)], [("AT", h)])
                S.add("pe", lambda e, A=A, X0=X0: e.matmul(A[:, 0:128], X0[:, 128:256], identb[:], start=True, stop=True), [("XM", h, 0), "identb"], [("ps", 2 * h, 0)])
                S.add("act", lambda e, A=A, X0=X0: e.activation(out=X0[:, 256:384], in_=A[:, 0:128], func=AF.Copy), [("ps", 2 * h, 0)], [("XM", h, 0)])
                S.add("pool", lambda e, X0=X0: e.tensor_copy(out=X0[:, 0:128], in_=identb[:]), ["identb"], [("XM", h, 0)])
                for it in range(6):
                    cur, nxt = XM[h][it % 2], XM[h][(it + 1) % 2]
                    ck, nk = [("XM", h, it % 2)], [("XM", h, (it + 1) % 2)]
                    S.add("pe", lambda e, Bk=Bk, cur=cur, it=it: e.matmul(Bk[:, 0:(256 if it < 5 else 128)], cur[:, 256:384], cur[:, 0:(256 if it < 5 else 128)],
                                                                         start=True, stop=True), ck, [("ps", 2 * h + 1, 0), ("ps", 2 * h + 1, 1)])
                    if it < 5:
                        S.add("pe", lambda e, Bk=Bk, cur=cur: e.matmul(Bk[:, 256:384], cur[:, 128:256], cur[:, 256:384], start=True, stop=True),
                              ck, [("ps", 2 * h + 1, 2)])
                        S.add("act", lambda e, Bk=Bk, nxt=nxt: e.activation(out=nxt[:, 128:384], in_=Bk[:, 128:384], func=AF.Copy),
                              [("ps", 2 * h + 1, 1), ("ps", 2 * h + 1, 2)], nk)
                    tt("dve", nxt[:, 0:128], Bk[:, 0:128], cur[:, 0:128], ALU.add, [("ps", 2 * h + 1, 0)] + ck, nk)
                Xf = XM[h][0][:, 0:128]
                ktok = kvtok[:, t, h * 64:(h + 1) * 64]
                S.add("pool", lambda e, h=h, ktok=ktok: e.tensor_scalar(out=kgb[h][:, 64 * (h % 2):64 * (h % 2) + 64], in0=ktok, scalar1=tsm[:, 4 + h:5 + h], scalar2=0.0, op0=ALU.mult, op1=ALU.add),
                      [("kvtok", t)] + TK, [("kg", h)])
                S.add("pool", lambda e, h=h, ktok=ktok: e.tensor_scalar(out=kdec[h][:, 64 * (h % 2):64 * (h % 2) + 64], in0=ktok, scalar1=tsm[:, 8 + h:9 + h], scalar2=0.0, op0=ALU.mult, op1=ALU.add),
                      [("kvtok", t)] + TK, [("kdec", h)])
                S.add("pe", lambda e, A=A, h=h, Xf=Xf, pb=pb: e.matmul(A[:, 0:128], kgb[h], Xf, start=True, stop=True),
                      [("kg", h), ("XM", h, 0)], [("ps", 2 * h, 0)])
                S.add("act", lambda e, A=A, h=h, pb=pb: e.activation(out=negwT[h][pb:pb + 64, :], in_=A[pb:pb + 64, 0:128], func=AF.Copy, scale=-1.0),
                      [("ps", 2 * h, 0)], [("negwT", h)])
                tt("pool", qgT[h][pb:pb + 64, :], qTh, Egc[h][pb:pb + 64, :], ALU.mult, [("qkT", 0), ("qkT", 1), ("Egc", h)], [("qgT", h)])

        def scan_chunk(t, d, c, h, fin):
            pb, hp, col = 64 * (h % 2), h // 2, 4 * d + h
            A = ps[2 * h]
            r = slice(64 * c, 64 * c + 64)
            Xf = XM[h][0][:, 0:128]
            sk, s16k = [("s", d, h)], [("s16", d, h)]
            vtok = kvtok[r, t, 256 + h * 64:256 + (h + 1) * 64]
            s16h = s16[pb:pb + 64, d, hp, :]
            s16f = s16[:, d, hp, :]
            s16fk = [("s16", d, 2 * hp), ("s16", d, 2 * hp + 1)]
            s32h = s32[pb:pb + 64, d, hp, :]
            vn = vnew[h][c]
            vk = [("vnew", h, c)]
            PV, PO, PS_, PSf = A[:, 128:192], A[:, 192:256], A[pb:pb + 64, 256:320], A[:, 256:320]
            S.add("pe", lambda e: e.matmul(PV, Xf, kvtok[:, t, 256 + h * 64:256 + (h + 1) * 64], start=True, stop=False), [("XM", h, 0), ("kvtok", t)], [("ps", 2 * h, 1)])
            chk(71)
            S.add("pe", lambda e: e.matmul(PV, negwT[h], s16f, start=False, stop=True), [("negwT", h)] + s16fk, [("ps", 2 * h, 1)])
            chk(72)
            S.add("act", lambda e: e.activation(out=vn[r, :], in_=A[r, 128:192], func=AF.Copy, scale=bq[r, t, col:col + 1]),
                  [("ps", 2 * h, 1), ("gb", t)], vk)
            chk(73)
            S.add("pe", lambda e: e.matmul(PO, qgT[h], s16f, start=True, stop=False), [("qgT", h)] + s16fk, [("ps", 2 * h, 1)])
            chk(74)
            S.add("pe", lambda e: e.matmul(PO, ATb[h], vn, start=False, stop=True), [("AT", h)] + vk, [("ps", 2 * h, 1)])
            chk(75)
            S.add("pe", lambda e: e.matmul(PSf, kdec[h], vn, start=True, stop=True), [("kdec", h)] + vk, [("ps", 2 * h, 2)])
            chk(76)
            if not fin:
                S.add("act", lambda e: e.activation(out=of_ap[r, t, h * 64:(h + 1) * 64], in_=A[r, 192:256], func=AF.Copy),
                      [("ps", 2 * h, 1)], [("of", t)])
                chk(77)
            else:
                S.add("act", lambda e: e.activation(out=otmp[h][r, :], in_=A[r, 192:256], func=AF.Copy), [("ps", 2 * h, 1)], [("otmp", h)])
                tt("dve", osum[r, h * 64:(h + 1) * 64], otmp[h][r, :], of_ap[r, t, h * 64:(h + 1) * 64], ALU.add,
                   [("otmp", h), ("of", t)], [("osum", h)])
                chk(78)
            clast = 64 * c + 63 if d == 0 else 64 * c
            S.add("dve", lambda e: e.tensor_scalar(out=s32h, in0=s32h, scalar1=Egc[h][pb:pb + 64, clast:clast + 1], scalar2=None, op0=ALU.mult),
                  [("Egc", h)] + sk, sk)
            chk(79)
            S.add("act", lambda e: e.activation(out=tmpS[h][pb:pb + 64, :], in_=PS_, func=AF.Copy), [("ps", 2 * h, 2)], [("tmpS", h)])
            S.add("dve", lambda e: e.tensor_tensor(out=s32h, in0=tmpS[h][pb:pb + 64, :], in1=s32h, op=ALU.add), [("tmpS", h)] + sk, sk)
            chk(80)
            S.add("act", lambda e: e.activation(out=s16h, in_=s32h, func=AF.Copy), sk, s16k)
            chk(81)

        def finalize(t):
            r = 2 if t < 2 else b
            ok = [("osum", h) for h in range(4)]
            S.add("act", lambda e: e.activation(out=osq, in_=osum, func=AF.Square), ok, ["osq"])
            S.add("dve", lambda e: e.tensor_reduce(out=sm[:, 32:36], in_=osq.rearrange("p (h d) -> p h d", h=4), axis=AX.X, op=ALU.add),
                  ["osq"], [("sm", 32)])
            rsqrt_inplace(sm[:, 32:36], 1.0 / 64, [("sm", 32)])
            for h in range(4):
                S.add("dve", lambda e, h=h: e.scalar_tensor_tensor(out=yab[:, h * 64:(h + 1) * 64], in0=osum[:, h * 64:(h + 1) * 64], scalar=sm[:, 32 + h:33 + h],
                                                                  in1=rowc[:, 512 + h * 64:512 + (h + 1) * 64], op0=ALU.mult, op1=ALU.mult),
                      [("osum", h), ("sm", 32), "rowc"], ["yab"])
            n0 = t * 128
            part = 0 if t < 2 else (256 if t < 10 else 1280)
            S.add("sp", lambda e: e.dma_start(out=yTt[:, 2:8, :], in_=spill_d[b, 2:8, :, n0:n0 + 128].rearrange("c p n -> p c n")),
                  [("spill", b, c, part) for c in range(2, 8)], ["yTt"], dma=True)
            S.add("sp", lambda e: e.dma_start(out=gat[:, :, :], in_=spill_d[b, 0:2, :, n0:n0 + 128].rearrange("c p n -> p c n")),
                  [("spill", b, c, part) for c in range(0, 2)], ["gat"], dma=True)
            for j in range(2):
                S.add("pe", lambda e, j=j: e.matmul(ps[3][:, j * 128:(j + 1) * 128], yab[:, j * 128:(j + 1) * 128], identb[:], start=True, stop=True),
                      ["yab", "identb"], [("ps", 3, j)])
                tt("dve", yTt[:, j, :], ps[3][:, j * 128:(j + 1) * 128], gat[:, j, :], ALU.mult, [("ps", 3, j), "gat"], ["yTt"])
            for nh in range(2):
                for kc in range(8):
                    S.add("pe", lambda e, nh=nh, kc=kc: e.matmul(ps[5 + 2 * nh][:, :], yTt[:, kc, :], wout[:, kc, nh * 512:(nh + 1) * 512],
                                                                start=(kc == 0), stop=(kc == 7)), ["yTt", ("wout", kc)], pk(5 + 2 * nh))
            load_xt(l, b, t, 0)
            for nh in range(2):
                tt("dve", rt[:, nh * 512:(nh + 1) * 512], ps[5 + 2 * nh][:, :], gateB[:, r, nh * 512:(nh + 1) * 512], ALU.mult,
                   pk(5 + 2 * nh) + [("gateB", r)], ["rt"])
            tt("pool", rt[:, :], rt[:, :], xt[:, 0, :], ALU.add, ["rt"] + XTK(0), ["rt"])
            if last:
                S.add("act", lambda e: e.activation(out=junk[:], in_=rt[:, :], func=AF.Square, accum_out=sm[:, 40:41]), ["rt"], ["junk", ("sm", 40)])
                rsqrt_inplace(sm[:, 40:41], 1.0 / 1024, [("sm", 40)])
                S.add("dve", lambda e: e.scalar_tensor_tensor(out=rt[:, :], in0=rt[:, :], scalar=sm[:, 40:41], in1=fgB[:], op0=ALU.mult, op1=ALU.mult),
                      ["rt", ("sm", 40), "fgB"], ["rt"])
            dsts = xdst(l, b, t, last)
            for i_, (dst, psl) in enumerate(dsts):
                if t < 2:
                    wk = [("cr", b, t)]
                elif len(dsts) == 1:
                    wk = [("xr", b, t - 2, j_) for j_ in range(4)]
                else:
                    wk = [("xr", b, t - 2, i_)]
                if last:
                    wk = [("yout", b, t, i_)]
                    out_keys.append(wk[0])
                S.add("sp", lambda e, dst=dst, psl=psl: e.dma_start(out=dst, in_=rt[psl, :]), ["rt"], wk, dma=True)

        segs = [(0, [0, 1], False), (1, [1, 0], True), (0, list(range(2, NT)), False), (1, list(range(NT - 1, 1, -1)), True)]
        for (d, tiles, fin) in segs:
            for t in tiles:
                prep_tile(t, d)
                chk(6)
                for c in ((0, 1) if d == 0 else (1, 0)):
                    for h in range(4):
                        scan_chunk(t, d, c, h, fin)
                chk(7)
                if fin and not (last and t < 2):
                    finalize(t)
                    chk(8)
        full_barrier(["rt", "yTt", "gat"] + XTK(0) + XTK(1))

    try:
      for l in range(nl):
          cur_l[0] = l
          last = l == NL - 1
          layer_prologue(l, last)
          chk(1)
          for b in range(nb):
              batch_body(l, b, last)
    except _Stop:
        pass
    fin_reads = out_keys + [("tap", n) for n in tap_d] + [("xr", b_, t_, i_) for b_ in range(nb) for t_ in range(16) for i_ in range(4)] + [("cr", b_, t_) for b_ in range(nb) for t_ in range(2)]
    S.add("pool", lambda e: e.memset(sm[:, 60:61], 0.0), fin_reads, [("sm", 60)])
    S.emit(nc, es)
    es.close()
    return nc


def _chunk_cols():
    cols = np.zeros((NCH, 128), np.int64) - 1
    for cc in range(NCH):
        for j in range(128):
            if cc < 6:
                cols[cc, j] = cc * 128 + j
            elif cc == 6:
                cols[cc, j] = 768 + j if j < 16 else -1
            else:
                cols[cc, j] = 784 + (cc - 7) * 128 + j
    return cols


def _consts():
    idx = np.arange(128)
    same = (idx[:, None] // 64) == (idx[None, :] // 64)
    c = np.zeros((128, 9, 128), np.float32)
    c[:, 0] = np.eye(128)
    c[:, 1] = 1.0
    for d in range(2):
        fwd = d == 0
        tri = same & ((idx[:, None] <= idx[None, :]) if fwd else (idx[:, None] >= idx[None, :]))
        allowed = same & ((idx[None, :] >= idx[:, None]) if fwd else (idx[None, :] <= idx[:, None]))
        c[:, 2 + d] = tri
        c[:, 4 + d] = np.where(allowed, 0.0, -30000.0)
        c[:, 6 + d] = allowed & (idx[None, :] != idx[:, None])
    c[:, 8] = same
    return c


def prep_inputs(inp):
    f = lambda a: np.ascontiguousarray(a, dtype=np.float32)
    sh = {}
    w_ada = inp["w_ada"]
    sh["w_ada"] = f(w_ada.reshape(NL, 8, 128, 24, 128).transpose(0, 3, 2, 1, 4))
    sh["b_adaT"] = f(inp["b_ada"].reshape(NL, 24, 128).transpose(0, 2, 1))
    sh["norm_gT"] = f(inp["norm_g"].reshape(NL, 8, 128).transpose(0, 2, 1))
    cols = _chunk_cols()
    w_in = inp["w_in"]
    wpad = np.concatenate([w_in, np.zeros((NL, 1024, 1), np.float32)], axis=2)
    wsel = wpad[:, :, cols.reshape(-1)].reshape(NL, 8, 128, NCH, 128)
    sh["w_in"] = f(wsel.transpose(0, 3, 2, 1, 4))
    sh["qkvw"] = f(inp["qkv_conv_w"].reshape(NL, 3, 6, 128).transpose(0, 3, 2, 1))
    sh["scw"] = f(inp["short_conv_w"].reshape(NL, 3, 2, 128).transpose(0, 3, 2, 1))
    sh["cfw"] = f(inp["conf_conv_w"].reshape(NL, 31, 2, 128).transpose(0, 3, 2, 1))
    cv = np.stack([inp["conf_conv_b"], inp["conf_ln_g"], inp["conf_ln_b"]], axis=1)
    sh["cvec"] = f(cv.reshape(NL, 3, 2, 128).transpose(0, 3, 1, 2))
    rowc = np.concatenate([inp["smlp_ln_g"], inp["smlp_ln_b"], np.tile(inp["gdn_norm_g"], (1, 4)),
                           inp["a_log"].reshape(NL, 8), inp["dt_bias"].reshape(NL, 8)], axis=1)
    sh["rowc"] = f(np.broadcast_to(rowc[:, None, :], (NL, 128, 784)))
    sh["wsT"] = f(inp["smlp_w"].transpose(0, 3, 1, 2))
    bs = inp["smlp_b"].reshape(NL, 2, 2, 1, 128)
    sh["bsT"] = f(np.broadcast_to(bs, (NL, 2, 2, 64, 128)).reshape(NL, 2, 128, 128).transpose(0, 2, 1, 3))
    sh["w_out"] = f(inp["w_out"].reshape(NL, 8, 128, 1024).transpose(0, 2, 1, 3))
    sh["final_gB"] = f(np.broadcast_to(inp["final_g"][None, :], (128, 1024)))
    sh["consts"] = _consts()
    maps = []
    for c in range(8):
        m = dict(sh)
        m["x"] = f(inp["x"][2 * c:2 * c + 2])
        m["ctx"] = f(inp["ctx"][2 * c:2 * c + 2])
        cc = np.stack([inp["c"][2 * c], inp["c"][2 * c + 1], inp["c_ctx"]], axis=1)
        m["ccat"] = f(cc.reshape(8, 128, 3).transpose(1, 0, 2))
        maps.append(m)
    return maps


def kernel(**inputs):
    inputs = {k: np.asarray(v) for k, v in inputs.items()}
    maps = prep_inputs(inputs)
    nc = build()
    res = run_bass_kernel_spmd(nc, maps, core_ids=list(range(8)))
    return np.concatenate([r["y"] for r in res.results], axis=0).astype(np.float32)
```

```python
from contextlib import ExitStack
import numpy as np
import concourse.bass as bass
import concourse.mybir as mybir
from concourse.bass_utils import run_bass_kernel_spmd

F32 = mybir.dt.float32
BF16 = mybir.dt.bfloat16
AF = mybir.ActivationFunctionType
ALU = mybir.AluOpType
AX = mybir.AxisListType

NL = 4
TAPL = 9
STOPL = 0
EPS = 1e-6
NT = 18
WP = 2352
NCH = 29
EPOCH = 4000
ATTACH = True
NRING = {"sp": 24, "pool": 8, "act": 4}
ENGS = ["pe", "act", "dve", "pool", "sp"]


def wcol(tok):
    return 16 + tok if tok < 256 else tok + 32


class Op:
    __slots__ = ("eng", "fn", "deps", "signal", "sig", "dma", "slot", "dval", "na")


class Sched:
    def __init__(self):
        self.ops = {e: [] for e in ENGS}
        self.lastw = {}
        self.rd = {}

    def add(self, eng, fn, reads=(), writes=(), dma=False, na=False):
        nk = lambda k: ("ps", k[1]) if (isinstance(k, tuple) and k[0] == "ps") else k
        reads = list(dict.fromkeys(nk(k) for k in reads))
        writes = list(dict.fromkeys(nk(k) for k in writes))
        op = Op()
        op.eng, op.fn, op.dma, op.signal, op.sig, op.slot, op.dval, op.na = eng, fn, dma, False, 0, 0, 0, na
        deps = []
        for k in reads:
            w = self.lastw.get(k)
            if w is not None:
                deps.append(w)
        for k in writes:
            w = self.lastw.get(k)
            if w is not None:
                deps.append(w)
            r = self.rd.get(k)
            if r:
                deps.extend(r[0].values())
                deps.extend(r[1])
        op.deps = [d for d in dict.fromkeys(deps) if d is not op]
        for d in op.deps:
            if not (d.eng == "pe" and eng == "pe"):
                d.signal = True
        for k in reads:
            r = self.rd.setdefault(k, ({}, []))
            if dma:
                r[1].append(op)
            else:
                r[0][eng] = op
        for k in writes:
            self.lastw[k] = op
            self.rd[k] = ({}, [])
        self.ops[eng].append(op)
        return op

    def emit(self, nc, es):
        nsig = {}
        for e in ENGS:
            cnt = 0
            dcnt = 0
            for op in self.ops[e]:
                if op.dma:
                    op.slot = dcnt % NRING[e]
                    op.dval = 16 * (dcnt // NRING[e] + 1)
                    dcnt += 1
                elif op.signal:
                    cnt += 1
                    op.sig = cnt
            nsig[e] = cnt
        csem = {e: [es.enter_context(nc.semaphore(f"c_{e}_{i}")) for i in range(nsig[e] // EPOCH + 1)]
                for e in ENGS if e != "sp"}
        dsem = {e: [es.enter_context(nc.semaphore(f"d_{e}_{i}")) for i in range(n)] for e, n in NRING.items()}
        block = es.enter_context(nc.Block())

        def run(e, eng):
            wabs = {}
            wd = {}
            for op in self.ops[e]:
                pend = []
                for d in op.deps:
                    if d.dma:
                        key = (d.eng, d.slot)
                        if wd.get(key, 0) >= d.dval:
                            continue
                        pend.append((dsem[d.eng][d.slot], d.dval))
                        wd[key] = d.dval
                    else:
                        if d.eng == "pe" and e == "pe":
                            continue
                        if wabs.get(d.eng, 0) >= d.sig:
                            continue
                        pend.append((csem[d.eng][(d.sig - 1) // EPOCH], (d.sig - 1) % EPOCH + 1))
                        wabs[d.eng] = d.sig
                if op.dma and op.dval > 16:
                    key = (e, op.slot)
                    if wd.get(key, 0) < op.dval - 16:
                        pend.append((dsem[e][op.slot], op.dval - 16))
                        wd[key] = op.dval - 16
                attach = None
                if pend and ATTACH and e in ("act", "dve", "pool") and not op.dma and not op.na:
                    attach = pend.pop()
                for (sem_, val_) in pend:
                    eng.wait_ge(sem_, val_)
                ins = op.fn(eng)
                if attach is not None:
                    ins._wait_ge(attach[0], attach[1])
                if op.dma:
                    ins.then_inc(dsem[e][op.slot], 16)
                elif op.signal:
                    ins.then_inc(csem[e][(op.sig - 1) // EPOCH], 1)

        @block.sync
        def _(eng):
            run("sp", eng)

        @block.gpsimd
        def _(eng):
            run("pool", eng)

        @block.scalar
        def _(eng):
            run("act", eng)

        @block.vector
        def _(eng):
            run("dve", eng)

        @block.tensor
        def _(eng):
            run("pe", eng)


class _Stop(Exception):
    pass


def build(nl=NL, nb=2, taps=None, dbg=False, stop=99):
    nc = bass.Bass("TRN2", target_bir_lowering=False)
    es = ExitStack()
    S = Sched()
    dram = lambda name, shape, dt=F32, kind="ExternalInput": nc.dram_tensor(name, list(shape), dt, kind=kind).ap()
    x_d = dram("x", [2, 2048, 1024])
    ctx_d = dram("ctx", [2, 256, 1024])
    ccat_d = dram("ccat", [128, 8, 3])
    consts_d = dram("consts", [128, 9, 128])
    wada_d = dram("w_ada", [NL, 24, 128, 8, 128])
    badaT_d = dram("b_adaT", [NL, 128, 24])
    normgT_d = dram("norm_gT", [NL, 128, 8])
    win_d = dram("w_in", [NL, NCH, 128, 8, 128])
    qkvw_d = dram("qkvw", [NL, 128, 6, 3])
    scw_d = dram("scw", [NL, 128, 2, 3])
    cfw_d = dram("cfw", [NL, 128, 2, 31])
    cvec_d = dram("cvec", [NL, 128, 3, 2])
    rowc_d = dram("rowc", [NL, 128, 784])
    wsT_d = dram("wsT", [NL, 128, 4, 128])
    bsT_d = dram("bsT", [NL, 128, 2, 128])
    wout_d = dram("w_out", [NL, 128, 8, 1024])
    fg_d = dram("final_gB", [128, 1024])
    y_d = dram("y", [2, 2048, 1024], F32, "ExternalOutput")
    xres_d = dram("xres", [2, 2048, 1024], F32, "ExternalOutput" if dbg else "Internal")
    cres_d = dram("cres", [2, 256, 1024], F32, "ExternalOutput" if dbg else "Internal")
    spill_d = dram("spill", [2, 8, 128, 2304], BF16, "Internal")
    tap_d = {}
    if taps:
        for name, shape in taps.items():
            tap_d[name] = dram("tap_" + name, shape, F32, "ExternalOutput")

    sb = lambda name, shape, dt=F32: es.enter_context(nc.sbuf_tensor("s_" + name, list(shape), dt))
    consts = sb("consts", [128, 9, 128])
    IDENT, ONES, TRI, NEGM, SMM, BO = consts[:, 0, :], consts[:, 1, :], (consts[:, 2, :], consts[:, 3, :]), \
        (consts[:, 4, :], consts[:, 5, :]), (consts[:, 6, :], consts[:, 7, :]), consts[:, 8, :]
    identb = sb("identb", [128, 128], BF16)
    wch = sb("wch", [128, 3, 8, 128], BF16)
    wout = sb("wout", [128, 8, 1024], BF16)
    gateB = sb("gateB", [128, 3, 1024])
    scT = sb("scT", [128, 8, 3])
    ccat = sb("ccat", [128, 8, 3])
    modT = sb("modT", [128, 24, 3])
    sc1T = sb("sc1T", [128, 8, 3])
    badaT = sb("badaT", [128, 24])
    normgT = sb("normgT", [128, 8])
    qkvw = sb("qkvw", [128, 6, 3])
    scw = sb("scw", [128, 2, 3])
    cfw = sb("cfw", [128, 2, 31])
    cvec = sb("cvec", [128, 3, 2])
    rowc = sb("rowc", [128, 784])
    negA = sb("negA", [128, 8])
    wsT32 = sb("wsT32", [128, 4, 128])
    wsT = sb("wsT", [128, 4, 128], BF16)
    bsT = sb("bsT", [128, 2, 128])
    fgB = sb("fgB", [128, 1024])
    xt = sb("xt", [128, 2, 1024])
    xn = sb("xn", [128, 2, 1024], BF16)
    junk = sb("junk", [128, 1024], BF16)
    sm = sb("sm", [128, 64])
    qT = sb("qT", [128, 2, WP], BF16)
    kT = sb("kT", [128, 2, WP], BF16)
    kvtok = sb("kvtok", [128, NT, 512], BF16)
    gq = sb("gq", [128, NT, 8])
    bq = sb("bq", [128, NT, 8])
    nbq = sb("nbq", [128, NT, 8])
    s32 = sb("s32", [128, 2, 2, 64])
    s16 = sb("s16", [128, 2, 2, 64], BF16)
    AE = 8 * 2304 + 5 * 2 * WP
    arena = sb("arena", [128, AE], BF16)
    hT = arena[:, 0:8 * 2304].rearrange("p (k n) -> p k n", k=8)
    WS = [arena[:, 8 * 2304 + i * 2 * WP: 8 * 2304 + (i + 1) * 2 * WP].bitcast(F32) for i in range(5)]
    _off = [0]

    def carve(n_el, dt=BF16):
        a = arena[:, _off[0]:_off[0] + n_el * (2 if dt == F32 else 1)]
        _off[0] += n_el * (2 if dt == F32 else 1)
        assert _off[0] <= 8 * 2304
        return a.bitcast(F32) if dt == F32 else a
    Gb = [carve(128, F32) for _ in range(4)]
    decT = [carve(128, F32) for _ in range(4)]
    decTs = [carve(128, F32) for _ in range(4)]
    Egc = [carve(128, F32) for _ in range(4)]
    XM = [[carve(384) for _ in range(2)] for _ in range(4)]
    ATb = [carve(128) for _ in range(4)]
    negwT = [carve(128) for _ in range(4)]
    qgT = [carve(128) for _ in range(4)]
    kgb = [carve(128) for _ in range(4)]
    kdec = [carve(128) for _ in range(4)]
    kTz = [carve(128) for _ in range(4)]
    tmpS = [carve(64, F32) for _ in range(4)]
    otmp = [carve(64, F32) for _ in range(4)]
    vnew = [[carve(64) for _ in range(2)] for _ in range(4)]
    tsm = carve(32, F32)
    osum = carve(256, F32)
    osq = carve(256, F32)
    yab = carve(256)
    yTt = carve(8 * 128).rearrange("p (k n) -> p k n", k=8)
    gat = carve(2 * 128).rearrange("p (k n) -> p k n", k=2)
    rt = carve(1024, F32)
    of_ap = arena[:, 8 * 2304: 8 * 2304 + 2 * NT * 256].bitcast(F32).rearrange("p (t n) -> p t n", t=NT)
    wa = arena[:, 8 * 2304 + 4 * WP: 8 * 2304 + 4 * WP + 2 * 2 * 1024].bitcast(F32).rearrange("p (i k n) -> p i k n", i=2, k=8)
    Dg = WS[4][:, 0:512]
    ps = [es.enter_context(nc.psum_tensor(f"ps{i}", [128, 512], F32)) for i in range(8)]

    def pk(b, r0=0, r1=4):
        return [("ps", b, r) for r in range(r0, r1)]

    ARK = ["arenaH"]
    WSK = lambda i: [("WS", i)]
    HK = lambda t0, t1: [("hT", t) for t in range(t0, t1)]

    S.add("sp", lambda e: e.dma_start(out=consts[:], in_=consts_d[:]), [], ["consts"], dma=True)
    S.add("sp", lambda e: e.dma_start(out=ccat[:], in_=ccat_d[:]), [], ["ccat"], dma=True)
    S.add("sp", lambda e: e.dma_start(out=fgB[:], in_=fg_d[:]), [], ["fgB"], dma=True)
    S.add("dve", lambda e: e.tensor_copy(out=identb[:], in_=IDENT), ["consts"], ["identb"])
    S.add("act", lambda e: e.activation(out=scT[:], in_=ccat[:], func=AF.Silu), ["ccat"], ["scT"])
    for i in range(5):
        S.add("pool", lambda e, i=i: e.memset(WS[i], 0.0), [], WSK(i))

    evac_rr = [0]
    out_keys = []
    bar_n = [0]
    bscr = sb("bscr", [128, 16])
    bdram = dram("bdram", [2, 128, 4], F32, "Internal")

    def full_barrier(extra_writes=()):
        n = bar_n[0]
        bar_n[0] += 1
        S.add("pe", lambda e: e.matmul(ps[0][:, 0:128], identb[:], identb[:], start=True, stop=True), ["identb"], [("bar1", n, "pe")] + pk(0))
        S.add("act", lambda e: e.activation(out=bscr[:, 0:1], in_=bscr[:, 8:9], func=AF.Copy), ["bscr8"], [("bar1", n, "act"), "bscr0"])
        S.add("dve", lambda e: e.tensor_copy(out=bscr[:, 1:2], in_=bscr[:, 8:9]), ["bscr8"], [("bar1", n, "dve"), "bscr1"])
        S.add("sp", lambda e: e.dma_start(out=bdram[0], in_=bscr[:, 8:12]), ["bscr8"], [("bar1", n, "sp"), "bdram0"], dma=True)
        S.add("pool", lambda e: e.memset(bscr[:, 2:3], 0.0), [("bar1", n, k) for k in ("pe", "act", "dve", "sp")],
              [("bar2", n), "bscr2"] + list(extra_writes))
        S.add("pe", lambda e: e.matmul(ps[0][:, 0:128], identb[:], identb[:], start=True, stop=True), [("bar2", n), "identb"], pk(0))
        S.add("act", lambda e: e.activation(out=bscr[:, 3:4], in_=bscr[:, 8:9], func=AF.Copy), [("bar2", n), "bscr8"], ["bscr3"])
        S.add("dve", lambda e: e.tensor_copy(out=bscr[:, 4:5], in_=bscr[:, 8:9]), [("bar2", n), "bscr8"], ["bscr4"])
        S.add("sp", lambda e: e.dma_start(out=bdram[1], in_=bscr[:, 8:12]), [("bar2", n), "bscr8"], ["bdram1"], dma=True)

    S.add("pool", lambda e: e.memset(bscr[:], 0.0), [], ["bscr8", "bscr0", "bscr1", "bscr2", "bscr3", "bscr4"])

    def evac_copy(dst, src, reads, writes, scale=None):
        evac_rr[0] ^= 1
        if evac_rr[0]:
            S.add("act", lambda e: e.activation(out=dst, in_=src, func=AF.Copy, scale=(1.0 if scale is None else scale)),
                  reads, writes)
        else:
            if scale is None:
                S.add("dve", lambda e: e.tensor_copy(out=dst, in_=src), reads, writes)
            else:
                S.add("dve", lambda e: e.tensor_scalar(out=dst, in0=src, scalar1=float(scale), scalar2=None, op0=ALU.mult),
                      reads, writes)

    def tap(name, ap, reads):
        if name in tap_d:
            S.add("pool", lambda e: e.dma_start(out=tap_d[name][:], in_=ap), reads, [("tap", name)], dma=True)

    wch_i = [0]
    psrot = [0]

    def load_wch(l, cc):
        i = wch_i[0] % 3
        wch_i[0] += 1
        S.add("pool", lambda e: e.dma_start(out=wch[:, i, :, :], in_=win_d[l, cc]), [], [("wch", i)], dma=True)
        return i

    GROUPS = [(0, 256)] + [(256 + 512 * g, 512) for g in range(4)]

    def proj_fm(l, cc, dst, dkeys, func=None):
        i = load_wch(l, cc)
        for (t0, n) in GROUPS:
            bnk = psrot[0] % 4
            psrot[0] += 1
            for kc in range(8):
                S.add("pe", lambda e, kc=kc, bnk=bnk, t0=t0, n=n: e.matmul(
                    ps[bnk][:, 0:n], wch[:, i, kc, :], hT[:, kc, t0:t0 + n], start=(kc == 0), stop=(kc == 7)),
                    [("wch", i)] + HK(t0 // 128, (t0 + n) // 128), pk(bnk))
            d = dst[:, wcol(t0):wcol(t0) + n]
            if func is None:
                evac_copy(d, ps[bnk][:, 0:n], pk(bnk), dkeys)
            else:
                S.add("act", lambda e, d=d, bnk=bnk, n=n: e.activation(out=d, in_=ps[bnk][:, 0:n], func=func),
                      pk(bnk), dkeys)

    R0, R1 = 16, 2336

    def zero_mid(i):
        S.add("pool", lambda e: e.memset(WS[i][:, 272:288], 0.0), [], WSK(i))

    def spill(b, c, i):
        for (c0, n0, n) in ((16, 0, 256), (288, 256, 1024), (1312, 1280, 1024)):
            S.add("pool", lambda e, c0=c0, n0=n0, n=n: e.dma_start(out=spill_d[b, c, :, n0:n0 + n], in_=WS[i][:, c0:c0 + n]),
                  WSK(i), [("spill", b, c, n0)], dma=True)

    def tt(eng, out, a, b_, op, reads, writes):
        S.add(eng, lambda e: e.tensor_tensor(out=out, in0=a, in1=b_, op=op), reads, writes)

    def conv_taps(dst, src, wts, ntap, bias, reads, writes):
        half = ntap // 2
        if bias is None:
            S.add("dve", lambda e: e.tensor_scalar(out=dst[:, R0:R1], in0=src[:, R0 - half:R1 - half], scalar1=wts[:, 0:1],
                                                   scalar2=None, op0=ALU.mult), reads, writes)
        else:
            S.add("dve", lambda e: e.tensor_scalar(out=dst[:, R0:R1], in0=src[:, R0 - half:R1 - half], scalar1=wts[:, 0:1],
                                                   scalar2=bias, op0=ALU.mult, op1=ALU.add), reads, writes)
        for k in range(1, ntap):
            S.add("dve", lambda e, k=k: e.scalar_tensor_tensor(out=dst[:, R0:R1], in0=src[:, R0 + k - half:R1 + k - half],
                                                               scalar=wts[:, k:k + 1], in1=dst[:, R0:R1], op0=ALU.mult, op1=ALU.add),
                  reads + writes, writes)

    def rsqrt_inplace(ap, scale, reads_writes):
        S.add("act", lambda e: e.activation(out=ap, in_=ap, func=AF.Sqrt, bias=epsc[:, 0:1], scale=scale), reads_writes + ["epsc"], reads_writes)
        S.add("dve", lambda e: e.reciprocal(out=ap, in_=ap), reads_writes, reads_writes)

    epsc = sb("epsc", [128, 2])
    S.add("pool", lambda e: e.memset(epsc[:, 0:1], EPS), [], ["epsc"])
    S.add("pool", lambda e: e.memset(epsc[:, 1:2], 1.0), ["epsc"], ["epsc"])

    def xsrc(l, b, t):
        if t < 2:
            base = ctx_d if l == 0 else cres_d
            return [(base[b, t * 128:(t + 1) * 128, :], slice(0, 128))]
        base = x_d if l == 0 else xres_d
        tt_ = t - 2
        if l % 2 == 0:
            return [(base[b, tt_ * 128:(tt_ + 1) * 128, :], slice(0, 128))]
        v = base[b].rearrange("(r c) d -> c r d", c=64)
        return [(v[4 * tt_ + i], slice(32 * i, 32 * i + 32)) for i in range(4)]

    def xdst(l, b, t, last):
        if t < 2:
            return [(cres_d[b, t * 128:(t + 1) * 128, :], slice(0, 128))]
        base = y_d if last else xres_d
        tt_ = t - 2
        if l % 2 == 0:
            return [(base[b, tt_ * 128:(tt_ + 1) * 128, :], slice(0, 128))]
        v = base[b].rearrange("(r c) d -> c r d", c=64)
        return [(v[4 * tt_ + i], slice(32 * i, 32 * i + 32)) for i in range(4)]

    def xkeys(b, t):
        return [("cr", b, t)] if t < 2 else [("xr", b, tt_, i_) for tt_ in range(16) for i_ in range(4)]

    def XTK(slot):
        return [("xt", slot, i_) for i_ in range(4)]

    def load_xt(l, b, t, slot):
        srcs = xsrc(l, b, t)
        for i_, (src, psl) in enumerate(srcs):
            S.add("sp", lambda e, src=src, psl=psl: e.dma_start(out=xt[psl, slot, :], in_=src),
                  (xkeys(b, t) if l > 0 else []), (XTK(slot) if len(srcs) == 1 else [("xt", slot, i_)]), dma=True)

    cur_l = [0]

    def chk(level):
        if stop == level and cur_l[0] == STOPL:
            raise _Stop()

    def layer_prologue(l, last):
        chk(10)
        for dst, src, key in ((badaT, badaT_d[l], "badaT"), (normgT, normgT_d[l], "normgT"), (qkvw, qkvw_d[l], "qkvw"),
                              (scw, scw_d[l], "scw"), (cfw, cfw_d[l], "cfw"), (cvec, cvec_d[l], "cvec"),
                              (rowc, rowc_d[l], "rowc"), (wsT32, wsT_d[l], "wsT32"), (bsT, bsT_d[l], "bsT")):
            S.add("sp", lambda e, dst=dst, src=src: e.dma_start(out=dst[:], in_=src), [], [key], dma=True)
        for h2 in range(8):
            S.add("pool", lambda e, h2=h2: e.dma_start(out=wout[:, h2, :], in_=wout_d[l, :, h2, :]),
                  [], [("wout", h2)], dma=True)
        S.add("dve", lambda e: e.tensor_copy(out=wsT[:], in_=wsT32[:]), ["wsT32"], ["wsT"])
        S.add("act", lambda e: e.activation(out=negA[:], in_=rowc[:, 768:776], func=AF.Exp), ["rowc"], ["negA"])
        S.add("dve", lambda e: e.tensor_scalar(out=negA[:], in0=negA[:], scalar1=-1.0, scalar2=None, op0=ALU.mult), ["negA"], ["negA"])
        chk(11)
        for cc in range(24):
            wi = cc % 2
            S.add("sp", lambda e, cc=cc, wi=wi: e.dma_start(out=wa[:, wi, :, :], in_=wada_d[l, cc]),
                  [], [("wa", wi)] + WSK(2) + WSK(3), dma=True)
            reg = 6 + cc % 2
            for kc in range(8):
                S.add("pe", lambda e, kc=kc, wi=wi, reg=reg: e.matmul(ps[reg][:, 0:3], wa[:, wi, kc, :], scT[:, kc, :],
                                                                    start=(kc == 0), stop=(kc == 7)),
                      [("wa", wi), "scT"] + WSK(2) + WSK(3), [("ps", reg, 0)])
            S.add("dve", lambda e, cc=cc, reg=reg: e.tensor_scalar(out=modT[:, cc, :], in0=ps[reg][:, 0:3],
                                                                   scalar1=badaT[:, cc:cc + 1], scalar2=None, op0=ALU.add),
                  [("ps", reg, 0), "badaT"], [("modT", cc)])
        for kc in range(8):
            S.add("dve", lambda e, kc=kc: e.tensor_scalar(out=sc1T[:, kc, :], in0=modT[:, 8 + kc, :], scalar1=1.0,
                                                          scalar2=normgT[:, kc:kc + 1], op0=ALU.add, op1=ALU.mult),
                  [("modT", 8 + kc), "normgT"], [("sc1T", kc)])
        for r in range(3):
            for hf in range(2):
                for j in range(4):
                    kc = hf * 4 + j
                    S.add("dve", lambda e, j=j, kc=kc, r=r: e.tensor_scalar(out=Dg[:, j * 128:(j + 1) * 128], in0=IDENT,
                                                                           scalar1=modT[:, 16 + kc, r:r + 1], scalar2=None, op0=ALU.mult),
                          [("modT", 16 + kc), "consts"], WSK(4))
                bnk = 4 + (r * 2 + hf) % 2
                S.add("pe", lambda e, bnk=bnk: e.matmul(ps[bnk][:, :], ONES, Dg, start=True, stop=True), WSK(4) + ["consts"], pk(bnk))
                evac_copy(gateB[:, r, hf * 512:(hf + 1) * 512], ps[bnk][:, :], pk(bnk), [("gateB", r)])

        if l == TAPL:
            tap("modT", modT[:].rearrange("p a b -> p (a b)"), [("modT", i_) for i_ in range(24)])
            tap("sc1T", sc1T[:].rearrange("p a b -> p (a b)"), [("sc1T", i_) for i_ in range(8)])
            tap("gateB", gateB[:, 2, :], [("gateB", 2)])

    def batch_body(l, b, last):
        for t in range(NT):
            slot = t % 2
            r = 2 if t < 2 else b
            load_xt(l, b, t, slot)
            xk = XTK(slot)
            if l == TAPL and b == 0 and t == 0:
                tap("xt0", xt[:, 0, :], xk)
            if l == TAPL and b == 0 and t == 2:
                tap("xt2", xt[:, 0, :], xk)
            S.add("act", lambda e, slot=slot: e.activation(out=junk[:], in_=xt[:, slot, :], func=AF.Square, accum_out=sm[:, slot:slot + 1]),
                  xk, ["junk", ("sm", slot)], na=True)
            rsqrt_inplace(sm[:, slot:slot + 1], 1.0 / 1024, [("sm", slot)])
            S.add("act", lambda e, slot=slot: e.activation(out=xn[:, slot, :], in_=xt[:, slot, :], func=AF.Copy, scale=sm[:, slot:slot + 1]),
                  xk + [("sm", slot)], [("xn", slot)])
            for hf in range(2):
                bnk = 4 + hf
                for j in range(4):
                    kc = hf * 4 + j
                    S.add("pe", lambda e, j=j, kc=kc, bnk=bnk, slot=slot: e.matmul(
                        ps[bnk][:, j * 128:(j + 1) * 128], xn[:, slot, kc * 128:(kc + 1) * 128], identb[:], start=True, stop=True),
                        [("xn", slot), "identb"], [("ps", bnk, j)])
                for j in range(4):
                    kc = hf * 4 + j
                    o_ = hT[:, kc, t * 128:(t + 1) * 128]
                    i_ = ps[bnk][:, j * 128:(j + 1) * 128]
                    rk = [("ps", bnk, j), ("sc1T", kc), ("modT", kc)]
                    if j % 2 == 0:
                        S.add("act", lambda e, o_=o_, i_=i_, kc=kc, r=r: e.activation(out=o_, in_=i_, func=AF.Identity,
                                                                                  scale=sc1T[:, kc, r:r + 1], bias=modT[:, kc, r:r + 1]),
                              rk, [("hT", t)])
                    else:
                        S.add("dve", lambda e, o_=o_, i_=i_, kc=kc, r=r: e.tensor_scalar(out=o_, in0=i_, scalar1=sc1T[:, kc, r:r + 1],
                                                                                      scalar2=modT[:, kc, r:r + 1], op0=ALU.mult, op1=ALU.add),
                              rk, [("hT", t)])

        if l == TAPL and b == 0:
            for i_ in range(2):
                tap(f"hT{i_}", hT[:, 0, i_ * 1152:(i_ + 1) * 1152], HK(0, NT))
        chk(2)
        for i_ in range(5):
            for (a_, b_) in ((0, 16), (272, 288), (2336, 2352)):
                S.add("pool", lambda e, i_=i_, a_=a_, b_=b_: e.memset(WS[i_][:, a_:b_], 0.0), [], WSK(i_))
        for j in range(2):
            proj_fm(l, 21 + j, WS[j], WSK(j), AF.Silu)
            spill(b, j, j)
        for j in range(2):
            zero_mid(0), zero_mid(1)
            proj_fm(l, 9 + j, WS[0], WSK(0))
            proj_fm(l, 11 + j, WS[1], WSK(1))
            tt("pool", WS[0][:, R0:R1], WS[0][:, R0:R1], WS[1][:, R0:R1], ALU.mult, WSK(0) + WSK(1), WSK(0))
            conv_taps(WS[1], WS[0], scw[:, j, :], 3, None, WSK(0) + ["scw"], WSK(1))
            proj_fm(l, 7 + j, WS[2], WSK(2))
            proj_fm(l, 23 + j, WS[3], WSK(3), AF.Silu)
            tt("pool", WS[2][:, R0:R1], WS[2][:, R0:R1], WS[1][:, R0:R1], ALU.mult, WSK(2) + WSK(1), WSK(2))
            tt("dve", WS[2][:, R0:R1], WS[2][:, R0:R1], WS[3][:, R0:R1], ALU.mult, WSK(2) + WSK(3), WSK(2))
            spill(b, 2 + j, 2)
        for j in range(2):
            zero_mid(0)
            proj_fm(l, 13 + j, WS[0], WSK(0))
            proj_fm(l, 15 + j, WS[1], WSK(1), AF.Sigmoid)
            tt("pool", WS[0][:, R0:R1], WS[0][:, R0:R1], WS[1][:, R0:R1], ALU.mult, WSK(0) + WSK(1), WSK(0))
            conv_taps(WS[2 + j], WS[0], cfw[:, j, :], 31, cvec[:, 0, j:j + 1], WSK(0) + ["cfw", "cvec"], WSK(2 + j))
        for (t0, n) in GROUPS:
            c0 = wcol(t0)
            sq = [WS[0][:, 0:512], WS[0][:, 512:1024]]
            mg, vg = WS[0][:, 1024:1536], WS[0][:, 1536:2048]
            for j in range(2):
                S.add("act", lambda e, j=j, c0=c0, n=n: e.activation(out=sq[j][:, 0:n], in_=WS[2 + j][:, c0:c0 + n], func=AF.Square),
                      WSK(2 + j), WSK(0))
            for j in range(2):
                S.add("pe", lambda e, j=j, c0=c0, n=n: e.matmul(ps[4][:, 0:n], ONES, WS[2 + j][:, c0:c0 + n], start=(j == 0), stop=(j == 1)),
                      WSK(2 + j) + ["consts"], pk(4))
            for j in range(2):
                S.add("pe", lambda e, j=j, n=n: e.matmul(ps[5][:, 0:n], ONES, sq[j][:, 0:n], start=(j == 0), stop=(j == 1)),
                      WSK(0) + ["consts"], pk(5))
            S.add("act", lambda e, n=n: e.activation(out=mg[:, 0:n], in_=ps[4][:, 0:n], func=AF.Copy, scale=1.0 / 256), pk(4), WSK(0))
            S.add("act", lambda e, n=n: e.activation(out=vg[:, 0:n], in_=ps[4][:, 0:n], func=AF.Square, scale=1.0 / 256), pk(4), WSK(0))
            S.add("dve", lambda e, n=n: e.scalar_tensor_tensor(out=vg[:, 0:n], in0=ps[5][:, 0:n], scalar=1.0 / 256, in1=vg[:, 0:n],
                                                              op0=ALU.mult, op1=ALU.subtract), pk(5) + WSK(0), WSK(0))
            rsqrt_inplace(vg[:, 0:n], 1.0, WSK(0))
            for j in range(2):
                z = WS[2 + j][:, c0:c0 + n]
                tt("dve", z, z, mg[:, 0:n], ALU.subtract, WSK(2 + j) + WSK(0), WSK(2 + j))
                tt("pool", z, z, vg[:, 0:n], ALU.mult, WSK(2 + j) + WSK(0), WSK(2 + j))
        for j in range(2):
            z = WS[2 + j][:, R0:R1]
            S.add("act", lambda e, z=z, j=j: e.activation(out=z, in_=z, func=AF.Silu, scale=cvec[:, 1, j:j + 1], bias=cvec[:, 2, j:j + 1]),
                  WSK(2 + j) + ["cvec"], WSK(2 + j))
            proj_fm(l, 25 + j, WS[1], WSK(1), AF.Silu)
            tt("dve", z, z, WS[1][:, R0:R1], ALU.mult, WSK(2 + j) + WSK(1), WSK(2 + j))
            spill(b, 4 + j, 2 + j)
        wiA = load_wch(l, 19)
        wiB = load_wch(l, 20)
        vt32 = WS[0][:, 0:256]
        vnz = WS[1][:, 0:256].bitcast(BF16)
        vnz3 = vnz.rearrange("p (j g x) -> p j g x", j=2, g=2)
        vt3 = vt32.rearrange("p (j g d) -> p j g d", j=2, g=2)
        lnb3 = rowc[:, 256:512].rearrange("p (j g d) -> p j g d", j=2, g=2)
        S.add("pool", lambda e: e.memset(WS[1][:, 0:256], 0.0), [], WSK(1))
        for t in range(NT):
            for jj, wi in ((0, wiA), (1, wiB)):
                for kc in range(8):
                    S.add("pe", lambda e, jj=jj, wi=wi, kc=kc, t=t: e.matmul(ps[4][:, jj * 128:(jj + 1) * 128], hT[:, kc, t * 128:(t + 1) * 128],
                                                                            wch[:, wi, kc, :], start=(kc == 0), stop=(kc == 7)),
                          [("wch", wi), ("hT", t)], pk(4, jj, jj + 1))
            S.add("act", lambda e: e.activation(out=vt32, in_=ps[4][:, 0:256], func=AF.Copy, accum_out=sm[:, 8:9]), pk(4, 0, 2), WSK(0) + [("sm", 8)], na=True)
            S.add("act", lambda e: e.activation(out=junk[:, 0:256], in_=ps[4][:, 0:256], func=AF.Square, accum_out=sm[:, 9:10]), pk(4, 0, 2), ["junk", ("sm", 9)], na=True)
            S.add("dve", lambda e: e.tensor_scalar(out=sm[:, 8:9], in0=sm[:, 8:9], scalar1=1.0 / 256, scalar2=None, op0=ALU.mult), [("sm", 8)], [("sm", 8)])
            S.add("dve", lambda e: e.tensor_tensor(out=sm[:, 10:11], in0=sm[:, 8:9], in1=sm[:, 8:9], op=ALU.mult), [("sm", 8)], [("sm", 10)])
            S.add("dve", lambda e: e.scalar_tensor_tensor(out=sm[:, 9:10], in0=sm[:, 9:10], scalar=1.0 / 256, in1=sm[:, 10:11], op0=ALU.mult, op1=ALU.subtract),
                  [("sm", 9), ("sm", 10)], [("sm", 9)])
            rsqrt_inplace(sm[:, 9:10], 1.0, [("sm", 9)])
            S.add("dve", lambda e: e.tensor_scalar(out=vt32, in0=vt32, scalar1=sm[:, 8:9], scalar2=sm[:, 9:10], op0=ALU.subtract, op1=ALU.mult),
                  WSK(0) + [("sm", 8), ("sm", 9)], WSK(0))
            tt("pool", vt32, vt32, rowc[:, 0:256], ALU.mult, WSK(0) + ["rowc"], WSK(0))
            for g2 in range(2):
                tt("dve", vnz3[:, :, g2, g2 * 64:(g2 + 1) * 64], vt3[:, :, g2, :], lnb3[:, :, g2, :], ALU.add, WSK(0) + ["rowc"], WSK(1))
            for j in range(2):
                for g2 in range(2):
                    g = 2 * j + g2
                    S.add("pe", lambda e, j=j, g2=g2, g=g: e.matmul(ps[5][:, j * 128:(j + 1) * 128], vnz3[:, j, g2, :],
                                                                   wsT[:, g, :], start=(g2 == 0), stop=(g2 == 1)),
                          WSK(1) + ["wsT"], pk(5, j, j + 1))
            for j in range(2):
                c0 = wcol(t * 128)
                tt("dve", WS[2 + j][:, c0:c0 + 128], ps[5][:, j * 128:(j + 1) * 128], bsT[:, j, :], ALU.add, pk(5, j, j + 1) + ["bsT"], WSK(2 + j))
        for j in range(2):
            proj_fm(l, 17 + j, WS[0], WSK(0))
            proj_fm(l, 27 + j, WS[1], WSK(1), AF.Silu)
            tt("pool", WS[0][:, R0:R1], WS[0][:, R0:R1], WS[2 + j][:, R0:R1], ALU.mult, WSK(0) + WSK(2 + j), WSK(0))
            tt("dve", WS[0][:, R0:R1], WS[0][:, R0:R1], WS[1][:, R0:R1], ALU.mult, WSK(0) + WSK(1), WSK(0))
            spill(b, 6 + j, 0)

        chk(3)
        for cc in range(6):
            src = WS[cc % 2]
            zero_mid(cc % 2)
            proj_fm(l, cc, src, WSK(cc % 2))
            conv_taps(WS[2], src, qkvw[:, cc, :], 3, None, WSK(cc % 2) + ["qkvw"], WSK(2))
            S.add("act", lambda e: e.activation(out=WS[2][:, R0:R1], in_=WS[2][:, R0:R1], func=AF.Silu), WSK(2), WSK(2))
            if cc < 4:
                dstT = qT if cc < 2 else kT
                S.add("act", lambda e: e.activation(out=WS[3][:, R0:R1], in_=WS[2][:, R0:R1], func=AF.Square), WSK(2), WSK(3))
                for (t0, n) in GROUPS:
                    c0 = wcol(t0)
                    S.add("pe", lambda e, c0=c0, n=n: e.matmul(ps[4][:, 0:n], BO, WS[3][:, c0:c0 + n], start=True, stop=True),
                          WSK(3) + ["consts"], pk(4))
                    rn = WS[4][:, 0:n]
                    S.add("act", lambda e, n=n, rn=rn: e.activation(out=rn, in_=ps[4][:, 0:n], func=AF.Sqrt, bias=epsc[:, 0:1], scale=1.0),
                          pk(4) + ["epsc"], WSK(4))
                    S.add("dve", lambda e, rn=rn: e.reciprocal(out=rn, in_=rn), WSK(4), WSK(4))
                    S.add("dve", lambda e, c0=c0, n=n, rn=rn, dstT=dstT, cc=cc: e.scalar_tensor_tensor(
                        out=dstT[:, cc % 2, c0:c0 + n], in0=WS[2][:, c0:c0 + n], scalar=(0.125 if cc < 2 else 1.0), in1=rn,
                        op0=ALU.mult, op1=ALU.mult), WSK(2) + WSK(4), [("qkT", cc)])
            if cc >= 2:
                for t in range(NT):
                    c0 = wcol(t * 128)
                    bnk = 5 + t % 2
                    if cc < 4:
                        S.add("pe", lambda e, c0=c0, bnk=bnk, cc=cc: e.matmul(ps[bnk][:, 0:128], kT[:, cc % 2, c0:c0 + 128], identb[:], start=True, stop=True),
                              [("qkT", cc), "identb"], pk(bnk, 0, 1))
                    else:
                        S.add("pe", lambda e, c0=c0, bnk=bnk: e.matmul(ps[bnk][:, 0:128], WS[2][:, c0:c0 + 128], IDENT, start=True, stop=True),
                              WSK(2) + ["consts"], pk(bnk, 0, 1))
                    evac_copy(kvtok[:, t, (cc - 2) * 128:(cc - 1) * 128], ps[bnk][:, 0:128], pk(bnk, 0, 1), [("kvtok", t)])
        wi = load_wch(l, 6)
        for t in range(NT):
            bnk = 5 + t % 2
            for kc in range(8):
                S.add("pe", lambda e, kc=kc, t=t, bnk=bnk: e.matmul(ps[bnk][:, 128:144], hT[:, kc, t * 128:(t + 1) * 128], wch[:, wi, kc, 0:16],
                                                                   start=(kc == 0), stop=(kc == 7)),
                      [("wch", wi), ("hT", t)], pk(bnk, 1, 2))
            tmp = sm[:, 16:24]
            tt("dve", tmp, ps[bnk][:, 128:136], rowc[:, 776:784], ALU.add, pk(bnk, 1, 2) + ["rowc"], [("sm", 16)])
            S.add("act", lambda e, tmp=tmp: e.activation(out=tmp, in_=tmp, func=AF.Exp), [("sm", 16)], [("sm", 16)])
            S.add("act", lambda e, tmp=tmp: e.activation(out=tmp, in_=tmp, func=AF.Ln, bias=epsc[:, 1:2], scale=1.0), [("sm", 16), "epsc"], [("sm", 16)])
            tt("dve", gq[:, t, :], tmp, negA[:], ALU.mult, [("sm", 16), "negA"], [("gb", t)])
            S.add("act", lambda e, t=t, bnk=bnk: e.activation(out=bq[:, t, :], in_=ps[bnk][:, 136:144], func=AF.Sigmoid), pk(bnk, 1, 2), [("gb", t)])
            S.add("dve", lambda e, t=t: e.tensor_scalar(out=nbq[:, t, :], in0=bq[:, t, :], scalar1=-1.0, scalar2=None, op0=ALU.mult), [("gb", t)], [("gb", t)])

        chk(4)
        full_barrier()
        for h_ in range(4):
            S.add("pool", lambda e, h_=h_: e.memset(negwT[h_], 0.0), [], [("negwT", h_)])
            S.add("pool", lambda e, h_=h_: e.memset(qgT[h_], 0.0), [], [("qgT", h_)])
            S.add("pool", lambda e, h_=h_: e.memset(kgb[h_], 0.0), [], [("kg", h_)])
            S.add("pool", lambda e, h_=h_: e.memset(kdec[h_], 0.0), [], [("kdec", h_)])
            S.add("pool", lambda e, h_=h_: e.memset(kTz[h_], 0.0), [], [("kTz", h_)])
            for c_ in range(2):
                S.add("pool", lambda e, h_=h_, c_=c_: e.memset(vnew[h_][c_], 0.0), [], [("vnew", h_, c_)])
        chk(5)
        S.add("pool", lambda e: e.memset(s32[:], 0.0), [], [("s", d, h) for d in range(2) for h in range(4)])
        S.add("pool", lambda e: e.memset(s16[:], 0.0), [], [("s16", d, h) for d in range(2) for h in range(4)])
        allH = HK(0, NT)

        def prep_tile(t, d):
            c0 = wcol(t * 128)
            pg = ps[1][:, 384:392]
            S.add("pe", lambda e: e.matmul(ps[1][:, 384:388], TRI[d], gq[:, t, 4 * d:4 * d + 4], start=True, stop=True), [("gb", t), "consts"], [("ps", 1, 3)])
            S.add("pe", lambda e: e.matmul(ps[1][:, 388:392], BO, gq[:, t, 4 * d:4 * d + 4], start=True, stop=True), [("gb", t), "consts"], [("ps", 1, 3)])
            TK = ["tsm"]
            S.add("dve", lambda e: e.tensor_scalar(out=tsm[:, 0:4], in0=ps[1][:, 384:388], scalar1=-1.0, scalar2=None, op0=ALU.mult),
                  [("ps", 1, 3)], TK)
            S.add("act", lambda e: e.activation(out=tsm[:, 4:8], in_=ps[1][:, 384:388], func=AF.Exp), [("ps", 1, 3)], TK)
            tt("dve", tsm[:, 12:16], ps[1][:, 388:392], tsm[:, 0:4], ALU.add, [("ps", 1, 3)] + TK, TK)
            S.add("act", lambda e: e.activation(out=tsm[:, 8:12], in_=tsm[:, 12:16], func=AF.Exp), TK, TK)
            for h in range(4):
                pb, hp, col = 64 * (h % 2), h // 2, 4 * d + h
                A, Bk = ps[2 * h], ps[2 * h + 1]
                kTh = kT[pb:pb + 64, hp, c0:c0 + 128]
                qTh = qT[pb:pb + 64, hp, c0:c0 + 128]
                hk = [("H", h)]
                S.add("pool", lambda e, h=h, pb=pb, kTh=kTh: e.tensor_copy(out=kTz[h][pb:pb + 64, :], in_=kTh), [("qkT", 2), ("qkT", 3)], [("kTz", h)])
                kTf = kT[:, hp, c0:c0 + 128]
                qTf = qT[:, hp, c0:c0 + 128]
                S.add("pe", lambda e, A=A, h=h, kTf=kTf: e.matmul(A[:, 0:128], kTz[h], kTf, start=True, stop=True), [("kTz", h), ("qkT", 2), ("qkT", 3)], [("ps", 2 * h, 0)])
                S.add("pe", lambda e, A=A, h=h, qTf=qTf: e.matmul(A[:, 128:256], kTz[h], qTf, start=True, stop=True),
                      [("kTz", h), ("qkT", 0), ("qkT", 1)], [("ps", 2 * h, 1)])
                S.add("pool", lambda e, h=h, col=col: e.tensor_scalar(out=Gb[h], in0=TRI[d], scalar1=gq[:, t, col:col + 1], scalar2=0.0, op0=ALU.mult, op1=ALU.add),
                      [("gb", t), "consts"], [("G", h)])
                S.add("pe", lambda e, A=A, h=h: e.matmul(A[:, 256:384], ONES, Gb[h], start=True, stop=False), [("G", h), "consts"], [("ps", 2 * h, 2)])
                S.add("pe", lambda e, A=A: e.matmul(A[:, 256:384], IDENT, NEGM[d], start=False, stop=True), ["consts"], [("ps", 2 * h, 2)])
                S.add("pe", lambda e, A=A, h=h: e.matmul(A[:, 384:512], ONES, Gb[h], start=True, stop=True), [("G", h), "consts"], [("ps", 2 * h, 3)])
                S.add("act", lambda e, A=A, h=h: e.activation(out=decT[h], in_=A[:, 256:384], func=AF.Exp, bias=tsm[:, h:h + 1], scale=1.0),
                      [("ps", 2 * h, 2)] + TK, [("decT", h)])
                S.add("act", lambda e, A=A, h=h: e.activation(out=Egc[h], in_=A[:, 384:512], func=AF.Exp), [("ps", 2 * h, 3)], [("Egc", h)])
                tt("pool", decTs[h], decT[h], SMM[d], ALU.mult, [("decT", h), "consts"], [("decTs", h)])
                X0 = XM[h][0]
                S.add("dve", lambda e, A=A, X0=X0, h=h, col=col: e.scalar_tensor_tensor(out=X0[:, 128:256], in0=A[:, 0:128], scalar=nbq[:, t, col:col + 1],
                                                                                   in1=decTs[h], op0=ALU.mult, op1=ALU.mult),
                      [("ps", 2 * h, 0), ("gb", t), ("decTs", h)], [("XM", h, 0)])
                tt("dve", ATb[h], A[:, 128:256], decT[h], ALU.mult, [("ps", 2 * h, 1), ("decT", h)], [("AT", h)])
                S.add("pe", lambda e, A=A, X0=X0: e.matmul(A[:, 0:128], X0[:, 128:256], identb[:], start=True, stop=True), [("XM", h, 0), "identb"], [("ps", 2 * h, 0)])
                S.add("act", lambda e, A=A, X0=X0: e.activation(out=X0[:, 256:384], in_=A[:, 0:128], func=AF.Copy), [("ps", 2 * h, 0)], [("XM", h, 0)])
                S.add("pool", lambda e, X0=X0: e.tensor_copy(out=X0[:, 0:128], in_=identb[:]), ["identb"], [("XM", h, 0)])
                for it in range(6):
                    cur, nxt = XM[h][it % 2], XM[h][(it + 1) % 2]
                    ck, nk = [("XM", h, it % 2)], [("XM", h, (it + 1) % 2)]
                    S.add("pe", lambda e, Bk=Bk, cur=cur, it=it: e.matmul(Bk[:, 0:(256 if it < 5 else 128)], cur[:, 256:384], cur[:, 0:(256 if it < 5 else 128)],
                                                                         start=True, stop=True), ck, [("ps", 2 * h + 1, 0), ("ps", 2 * h + 1, 1)])
                    if it < 5:
                        S.add("pe", lambda e, Bk=Bk, cur=cur: e.matmul(Bk[:, 256:384], cur[:, 128:256], cur[:, 256:384], start=True, stop=True),
                              ck, [("ps", 2 * h + 1, 2)])
                        S.add("act", lambda e, Bk=Bk, nxt=nxt: e.activation(out=nxt[:, 128:384], in_=Bk[:, 128:384], func=AF.Copy),
                              [("ps", 2 * h + 1, 1), ("ps", 2 * h + 1, 2)], nk)
                    tt("dve", nxt[:, 0:128], Bk[:, 0:128], cur[:, 0:128], ALU.add, [("ps", 2 * h + 1, 0)] + ck, nk)
                Xf = XM[h][0][:, 0:128]
                ktok = kvtok[:, t, h * 64:(h + 1) * 64]
                S.add("pool", lambda e, h=h, ktok=ktok: e.tensor_scalar(out=kgb[h][:, 64 * (h % 2):64 * (h % 2) + 64], in0=ktok, scalar1=tsm[:, 4 + h:5 + h], scalar2=0.0, op0=ALU.mult, op1=ALU.add),
                      [("kvtok", t)] + TK, [("kg", h)])
                S.add("pool", lambda e, h=h, ktok=ktok: e.tensor_scalar(out=kdec[h][:, 64 * (h % 2):64 * (h % 2) + 64], in0=ktok, scalar1=tsm[:, 8 + h:9 + h], scalar2=0.0, op0=ALU.mult, op1=ALU.add),
                      [("kvtok", t)] + TK, [("kdec", h)])
                S.add("pe", lambda e, A=A, h=h, Xf=Xf, pb=pb: e.matmul(A[:, 0:128], kgb[h], Xf, start=True, stop=True),
                      [("kg", h), ("XM", h, 0)], [("ps", 2 * h, 0)])
                S.add("act", lambda e, A=A, h=h, pb=pb: e.activation(out=negwT[h][pb:pb + 64, :], in_=A[pb:pb + 64, 0:128], func=AF.Copy, scale=-1.0),
                      [("ps", 2 * h, 0)], [("negwT", h)])
                tt("pool", qgT[h][pb:pb + 64, :], qTh, Egc[h][pb:pb + 64, :], ALU.mult, [("qkT", 0), ("qkT", 1), ("Egc", h)], [("qgT", h)])

        def scan_chunk(t, d, c, h, fin):
            pb, hp, col = 64 * (h % 2), h // 2, 4 * d + h
            A = ps[2 * h]
            r = slice(64 * c, 64 * c + 64)
            Xf = XM[h][0][:, 0:128]
            sk, s16k = [("s", d, h)], [("s16", d, h)]
            vtok = kvtok[r, t, 256 + h * 64:256 + (h + 1) * 64]
            s16h = s16[pb:pb + 64, d, hp, :]
            s16f = s16[:, d, hp, :]
            s16fk = [("s16", d, 2 * hp), ("s16", d, 2 * hp + 1)]
            s32h = s32[pb:pb + 64, d, hp, :]
            vn = vnew[h][c]
            vk = [("vnew", h, c)]
            PV, PO, PS_, PSf = A[:, 128:192], A[:, 192:256], A[pb:pb + 64, 256:320], A[:, 256:320]
            S.add("pe", lambda e: e.matmul(PV, Xf, kvtok[:, t, 256 + h * 64:256 + (h + 1) * 64], start=True, stop=False), [("XM", h, 0), ("kvtok", t)], [("ps", 2 * h, 1)])
            chk(71)
            S.add("pe", lambda e: e.matmul(PV, negwT[h], s16f, start=False, stop=True), [("negwT", h)] + s16fk, [("ps", 2 * h, 1)])
            chk(72)
            S.add("act", lambda e: e.activation(out=vn[r, :], in_=A[r, 128:192], func=AF.Copy, scale=bq[r, t, col:col + 1]),
                  [("ps", 2 * h, 1), ("gb", t)], vk)
            chk(73)
            S.add("pe", lambda e: e.matmul(PO, qgT[h], s16f, start=True, stop=False), [("qgT", h)] + s16fk, [("ps", 2 * h, 1)])
            chk(74)
            S.add("pe", lambda e: e.matmul(PO, ATb[h], vn, start=False, stop=True), [("AT", h)] + vk, [("ps", 2 * h, 1)])
            chk(75)
            S.add("pe", lambda e: e.matmul(PSf, kdec[h], vn, start=True, stop=True), [("kdec", h)] + vk, [("ps", 2 * h, 2)])
            chk(76)
            if not fin:
                S.add("act", lambda e: e.activation(out=of_ap[r, t, h * 64:(h + 1) * 64], in_=A[r, 192:256], func=AF.Copy),
                      [("ps", 2 * h, 1)], [("of", t)])
                chk(77)
            else:
                S.add("act", lambda e: e.activation(out=otmp[h][r, :], in_=A[r, 192:256], func=AF.Copy), [("ps", 2 * h, 1)], [("otmp", h)])
                tt("dve", osum[r, h * 64:(h + 1) * 64], otmp[h][r, :], of_ap[r, t, h * 64:(h + 1) * 64], ALU.add,
                   [("otmp", h), ("of", t)], [("osum", h)])
                chk(78)
            clast = 64 * c + 63 if d == 0 else 64 * c
            S.add("dve", lambda e: e.tensor_scalar(out=s32h, in0=s32h, scalar1=Egc[h][pb:pb + 64, clast:clast + 1], scalar2=None, op0=ALU.mult),
                  [("Egc", h)] + sk, sk)
            chk(79)
            S.add("act", lambda e: e.activation(out=tmpS[h][pb:pb + 64, :], in_=PS_, func=AF.Copy), [("ps", 2 * h, 2)], [("tmpS", h)])
            S.add("dve", lambda e: e.tensor_tensor(out=s32h, in0=tmpS[h][pb:pb + 64, :], in1=s32h, op=ALU.add), [("tmpS", h)] + sk, sk)
            chk(80)
            S.add("act", lambda e: e.activation(out=s16h, in_=s32h, func=AF.Copy), sk, s16k)
            chk(81)

        def finalize(t):
            r = 2 if t < 2 else b
            ok = [("osum", h) for h in range(4)]
            S.add("act", lambda e: e.activation(out=osq, in_=osum, func=AF.Square), ok, ["osq"])
            S.add("dve", lambda e: e.tensor_reduce(out=sm[:, 32:36], in_=osq.rearrange("p (h d) -> p h d", h=4), axis=AX.X, op=ALU.add),
                  ["osq"], [("sm", 32)])
            rsqrt_inplace(sm[:, 32:36], 1.0 / 64, [("sm", 32)])
            for h in range(4):
                S.add("dve", lambda e, h=h: e.scalar_tensor_tensor(out=yab[:, h * 64:(h + 1) * 64], in0=osum[:, h * 64:(h + 1) * 64], scalar=sm[:, 32 + h:33 + h],
                                                                  in1=rowc[:, 512 + h * 64:512 + (h + 1) * 64], op0=ALU.mult, op1=ALU.mult),
                      [("osum", h), ("sm", 32), "rowc"], ["yab"])
            n0 = t * 128
            part = 0 if t < 2 else (256 if t < 10 else 1280)
            S.add("sp", lambda e: e.dma_start(out=yTt[:, 2:8, :], in_=spill_d[b, 2:8, :, n0:n0 + 128].rearrange("c p n -> p c n")),
                  [("spill", b, c, part) for c in range(2, 8)], ["yTt"], dma=True)
            S.add("sp", lambda e: e.dma_start(out=gat[:, :, :], in_=spill_d[b, 0:2, :, n0:n0 + 128].rearrange("c p n -> p c n")),
                  [("spill", b, c, part) for c in range(0, 2)], ["gat"], dma=True)
            for j in range(2):
                S.add("pe", lambda e, j=j: e.matmul(ps[3][:, j * 128:(j + 1) * 128], yab[:, j * 128:(j + 1) * 128], identb[:], start=True, stop=True),
                      ["yab", "identb"], [("ps", 3, j)])
                tt("dve", yTt[:, j, :], ps[3][:, j * 128:(j + 1) * 128], gat[:, j, :], ALU.mult, [("ps", 3, j), "gat"], ["yTt"])
            for nh in range(2):
                for kc in range(8):
                    S.add("pe", lambda e, nh=nh, kc=kc: e.matmul(ps[5 + 2 * nh][:, :], yTt[:, kc, :], wout[:, kc, nh * 512:(nh + 1) * 512],
                                                                start=(kc == 0), stop=(kc == 7)), ["yTt", ("wout", kc)], pk(5 + 2 * nh))
            load_xt(l, b, t, 0)
            for nh in range(2):
                tt("dve", rt[:, nh * 512:(nh + 1) * 512], ps[5 + 2 * nh][:, :], gateB[:, r, nh * 512:(nh + 1) * 512], ALU.mult,
                   pk(5 + 2 * nh) + [("gateB", r)], ["rt"])
            tt("pool", rt[:, :], rt[:, :], xt[:, 0, :], ALU.add, ["rt"] + XTK(0), ["rt"])
            if last:
                S.add("act", lambda e: e.activation(out=junk[:], in_=rt[:, :], func=AF.Square, accum_out=sm[:, 40:41]), ["rt"], ["junk", ("sm", 40)], na=True)
                rsqrt_inplace(sm[:, 40:41], 1.0 / 1024, [("sm", 40)])
                S.add("dve", lambda e: e.scalar_tensor_tensor(out=rt[:, :], in0=rt[:, :], scalar=sm[:, 40:41], in1=fgB[:], op0=ALU.mult, op1=ALU.mult),
                      ["rt", ("sm", 40), "fgB"], ["rt"])
            dsts = xdst(l, b, t, last)
            for i_, (dst, psl) in enumerate(dsts):
                if t < 2:
                    wk = [("cr", b, t)]
                elif len(dsts) == 1:
                    wk = [("xr", b, t - 2, j_) for j_ in range(4)]
                else:
                    wk = [("xr", b, t - 2, i_)]
                if last:
                    wk = [("yout", b, t, i_)]
                    out_keys.append(wk[0])
                S.add("sp", lambda e, dst=dst, psl=psl: e.dma_start(out=dst, in_=rt[psl, :]), ["rt"], wk, dma=True)

        segs = [(0, [0, 1], False), (1, [1, 0], True), (0, list(range(2, NT)), False), (1, list(range(NT - 1, 1, -1)), True)]
        for (d, tiles, fin) in segs:
            for t in tiles:
                prep_tile(t, d)
                chk(6)
                for c in ((0, 1) if d == 0 else (1, 0)):
                    for h in range(4):
                        scan_chunk(t, d, c, h, fin)
                chk(7)
                if fin and not (last and t < 2):
                    finalize(t)
                    chk(8)
        full_barrier(["rt", "yTt", "gat"] + XTK(0) + XTK(1))

    try:
      for l in range(nl):
          cur_l[0] = l
          last = l == NL - 1
          layer_prologue(l, last)
          chk(1)
          for b in range(nb):
              batch_body(l, b, last)
    except _Stop:
        pass
    fin_reads = out_keys + [("tap", n) for n in tap_d] + [("xr", b_, t_, i_) for b_ in range(nb) for t_ in range(16) for i_ in range(4)] + [("cr", b_, t_) for b_ in range(nb) for t_ in range(2)]
    S.add("pool", lambda e: e.memset(sm[:, 60:61], 0.0), fin_reads, [("sm", 60)])
    S.emit(nc, es)
    es.close()
    return nc


def _chunk_cols():
    cols = np.zeros((NCH, 128), np.int64) - 1
    for cc in range(NCH):
        for j in range(128):
            if cc < 6:
                cols[cc, j] = cc * 128 + j
            elif cc == 6:
                cols[cc, j] = 768 + j if j < 16 else -1
            else:
                cols[cc, j] = 784 + (cc - 7) * 128 + j
    return cols


def _consts():
    idx = np.arange(128)
    same = (idx[:, None] // 64) == (idx[None, :] // 64)
    c = np.zeros((128, 9, 128), np.float32)
    c[:, 0] = np.eye(128)
    c[:, 1] = 1.0
    for d in range(2):
        fwd = d == 0
        tri = same & ((idx[:, None] <= idx[None, :]) if fwd else (idx[:, None] >= idx[None, :]))
        allowed = same & ((idx[None, :] >= idx[:, None]) if fwd else (idx[None, :] <= idx[:, None]))
        c[:, 2 + d] = tri
        c[:, 4 + d] = np.where(allowed, 0.0, -30000.0)
        c[:, 6 + d] = allowed & (idx[None, :] != idx[:, None])
    c[:, 8] = same
    return c


def prep_inputs(inp):
    f = lambda a: np.ascontiguousarray(a, dtype=np.float32)
    sh = {}
    w_ada = inp["w_ada"]
    sh["w_ada"] = f(w_ada.reshape(NL, 8, 128, 24, 128).transpose(0, 3, 2, 1, 4))
    sh["b_adaT"] = f(inp["b_ada"].reshape(NL, 24, 128).transpose(0, 2, 1))
    sh["norm_gT"] = f(inp["norm_g"].reshape(NL, 8, 128).transpose(0, 2, 1))
    cols = _chunk_cols()
    w_in = inp["w_in"]
    wpad = np.concatenate([w_in, np.zeros((NL, 1024, 1), np.float32)], axis=2)
    wsel = wpad[:, :, cols.reshape(-1)].reshape(NL, 8, 128, NCH, 128)
    sh["w_in"] = f(wsel.transpose(0, 3, 2, 1, 4))
    sh["qkvw"] = f(inp["qkv_conv_w"].reshape(NL, 3, 6, 128).transpose(0, 3, 2, 1))
    sh["scw"] = f(inp["short_conv_w"].reshape(NL, 3, 2, 128).transpose(0, 3, 2, 1))
    sh["cfw"] = f(inp["conf_conv_w"].reshape(NL, 31, 2, 128).transpose(0, 3, 2, 1))
    cv = np.stack([inp["conf_conv_b"], inp["conf_ln_g"], inp["conf_ln_b"]], axis=1)
    sh["cvec"] = f(cv.reshape(NL, 3, 2, 128).transpose(0, 3, 1, 2))
    rowc = np.concatenate([inp["smlp_ln_g"], inp["smlp_ln_b"], np.tile(inp["gdn_norm_g"], (1, 4)),
                           inp["a_log"].reshape(NL, 8), inp["dt_bias"].reshape(NL, 8)], axis=1)
    sh["rowc"] = f(np.broadcast_to(rowc[:, None, :], (NL, 128, 784)))
    sh["wsT"] = f(inp["smlp_w"].transpose(0, 3, 1, 2))
    bs = inp["smlp_b"].reshape(NL, 2, 2, 1, 128)
    sh["bsT"] = f(np.broadcast_to(bs, (NL, 2, 2, 64, 128)).reshape(NL, 2, 128, 128).transpose(0, 2, 1, 3))
    sh["w_out"] = f(inp["w_out"].reshape(NL, 8, 128, 1024).transpose(0, 2, 1, 3))
    sh["final_gB"] = f(np.broadcast_to(inp["final_g"][None, :], (128, 1024)))
    sh["consts"] = _consts()
    maps = []
    for c in range(8):
        m = dict(sh)
        m["x"] = f(inp["x"][2 * c:2 * c + 2])
        m["ctx"] = f(inp["ctx"][2 * c:2 * c + 2])
        cc = np.stack([inp["c"][2 * c], inp["c"][2 * c + 1], inp["c_ctx"]], axis=1)
        m["ccat"] = f(cc.reshape(8, 128, 3).transpose(1, 0, 2))
        maps.append(m)
    return maps


def kernel(**inputs):
    inputs = {k: np.asarray(v) for k, v in inputs.items()}
    maps = prep_inputs(inputs)
    nc = build()
    res = run_bass_kernel_spmd(nc, maps, core_ids=list(range(8)))
    return np.concatenate([r["y"] for r in res.results], axis=0).astype(np.float32)
```

```python
from contextlib import ExitStack
import numpy as np
import concourse.bass as bass
import concourse.mybir as mybir
from concourse.bass_utils import run_bass_kernel_spmd

F32 = mybir.dt.float32
BF16 = mybir.dt.bfloat16
AF = mybir.ActivationFunctionType
ALU = mybir.AluOpType
AX = mybir.AxisListType

NL = 4
TAPL = 9
STOPL = 0
EPS = 1e-6
NT = 18
WP = 2352
NCH = 29
EPOCH = 4000
ATTACH = True
NRING = {"sp": 24, "pool": 8, "act": 4}
ENGS = ["pe", "act", "dve", "pool", "sp"]


def wcol(tok):
    return 16 + tok if tok < 256 else tok + 32


class Op:
    __slots__ = ("eng", "fn", "deps", "signal", "sig", "dma", "slot", "dval", "na")


class Sched:
    def __init__(self):
        self.ops = {e: [] for e in ENGS}
        self.lastw = {}
        self.rd = {}

    def add(self, eng, fn, reads=(), writes=(), dma=False, na=False):
        nk = lambda k: ("ps", k[1]) if (isinstance(k, tuple) and k[0] == "ps") else k
        reads = list(dict.fromkeys(nk(k) for k in reads))
        writes = list(dict.fromkeys(nk(k) for k in writes))
        op = Op()
        op.eng, op.fn, op.dma, op.signal, op.sig, op.slot, op.dval, op.na = eng, fn, dma, False, 0, 0, 0, na
        deps = []
        for k in reads:
            w = self.lastw.get(k)
            if w is not None:
                deps.append(w)
        for k in writes:
            w = self.lastw.get(k)
            if w is not None:
                deps.append(w)
            r = self.rd.get(k)
            if r:
                deps.extend(r[0].values())
                deps.extend(r[1])
        op.deps = [d for d in dict.fromkeys(deps) if d is not op]
        for d in op.deps:
            if not (d.eng == "pe" and eng == "pe"):
                d.signal = True
        for k in reads:
            r = self.rd.setdefault(k, ({}, []))
            if dma:
                r[1].append(op)
            else:
                r[0][eng] = op
        for k in writes:
            self.lastw[k] = op
            self.rd[k] = ({}, [])
        self.ops[eng].append(op)
        return op

    def emit(self, nc, es):
        nsig = {}
        for e in ENGS:
            cnt = 0
            dcnt = 0
            for op in self.ops[e]:
                if op.dma:
                    op.slot = dcnt % NRING[e]
                    op.dval = 16 * (dcnt // NRING[e] + 1)
                    dcnt += 1
                elif op.signal:
                    cnt += 1
                    op.sig = cnt
            nsig[e] = cnt
        csem = {e: [es.enter_context(nc.semaphore(f"c_{e}_{i}")) for i in range(nsig[e] // EPOCH + 1)]
                for e in ENGS if e != "sp"}
        dsem = {e: [es.enter_context(nc.semaphore(f"d_{e}_{i}")) for i in range(n)] for e, n in NRING.items()}
        block = es.enter_context(nc.Block())

        def run(e, eng):
            wabs = {}
            wd = {}
            for op in self.ops[e]:
                pend = []
                for d in op.deps:
                    if d.dma:
                        key = (d.eng, d.slot)
                        if wd.get(key, 0) >= d.dval:
                            continue
                        pend.append((dsem[d.eng][d.slot], d.dval))
                        wd[key] = d.dval
                    else:
                        if d.eng == "pe" and e == "pe":
                            continue
                        if wabs.get(d.eng, 0) >= d.sig:
                            continue
                        pend.append((csem[d.eng][(d.sig - 1) // EPOCH], (d.sig - 1) % EPOCH + 1))
                        wabs[d.eng] = d.sig
                if op.dma and op.dval > 16:
                    key = (e, op.slot)
                    if wd.get(key, 0) < op.dval - 16:
                        pend.append((dsem[e][op.slot], op.dval - 16))
                        wd[key] = op.dval - 16
                attach = None
                if pend and ATTACH and e in ("act", "dve", "pool") and not op.dma and not op.na:
                    attach = pend.pop()
                for (sem_, val_) in pend:
                    eng.wait_ge(sem_, val_)
                ins = op.fn(eng)
                if attach is not None:
                    ins._wait_ge(attach[0], attach[1])
                if op.dma:
                    ins.then_inc(dsem[e][op.slot], 16)
                elif op.signal:
                    ins.then_inc(csem[e][(op.sig - 1) // EPOCH], 1)

        @block.sync
        def _(eng):
            run("sp", eng)

        @block.gpsimd
        def _(eng):
            run("pool", eng)

        @block.scalar
        def _(eng):
            run("act", eng)

        @block.vector
        def _(eng):
            run("dve", eng)

        @block.tensor
        def _(eng):
            run("pe", eng)


class _Stop(Exception):
    pass


def build(nl=NL, nb=2, taps=None, dbg=False, stop=99):
    nc = bass.Bass("TRN2", target_bir_lowering=False)
    es = ExitStack()
    S = Sched()
    dram = lambda name, shape, dt=F32, kind="ExternalInput": nc.dram_tensor(name, list(shape), dt, kind=kind).ap()
    x_d = dram("x", [2, 2048, 1024])
    ctx_d = dram("ctx", [2, 256, 1024])
    ccat_d = dram("ccat", [128, 8, 3])
    consts_d = dram("consts", [128, 9, 128])
    wada_d = dram("w_ada", [NL, 24, 128, 8, 128])
    badaT_d = dram("b_adaT", [NL, 128, 24])
    normgT_d = dram("norm_gT", [NL, 128, 8])
    win_d = dram("w_in", [NL, NCH, 128, 8, 128])
    qkvw_d = dram("qkvw", [NL, 128, 6, 3])
    scw_d = dram("scw", [NL, 128, 2, 3])
    cfw_d = dram("cfw", [NL, 128, 2, 31])
    cvec_d = dram("cvec", [NL, 128, 3, 2])
    rowc_d = dram("rowc", [NL, 128, 784])
    wsT_d = dram("wsT", [NL, 128, 4, 128])
    bsT_d = dram("bsT", [NL, 128, 2, 128])
    wout_d = dram("w_out", [NL, 128, 8, 1024])
    fg_d = dram("final_gB", [128, 1024])
    y_d = dram("y", [2, 2048, 1024], F32, "ExternalOutput")
    xres_d = dram("xres", [2, 2048, 1024], F32, "ExternalOutput" if dbg else "Internal")
    cres_d = dram("cres", [2, 256, 1024], F32, "ExternalOutput" if dbg else "Internal")
    spill_d = dram("spill", [2, 8, 128, 2304], BF16, "Internal")
    tap_d = {}
    if taps:
        for name, shape in taps.items():
            tap_d[name] = dram("tap_" + name, shape, F32, "ExternalOutput")

    sb = lambda name, shape, dt=F32: es.enter_context(nc.sbuf_tensor("s_" + name, list(shape), dt))
    consts = sb("consts", [128, 9, 128])
    IDENT, ONES, TRI, NEGM, SMM, BO = consts[:, 0, :], consts[:, 1, :], (consts[:, 2, :], consts[:, 3, :]), \
        (consts[:, 4, :], consts[:, 5, :]), (consts[:, 6, :], consts[:, 7, :]), consts[:, 8, :]
    identb = sb("identb", [128, 128], BF16)
    wch = sb("wch", [128, 3, 8, 128], BF16)
    wout = sb("wout", [128, 8, 1024], BF16)
    gateB = sb("gateB", [128, 3, 1024])
    scT = sb("scT", [128, 8, 3])
    ccat = sb("ccat", [128, 8, 3])
    modT = sb("modT", [128, 24, 3])
    sc1T = sb("sc1T", [128, 8, 3])
    badaT = sb("badaT", [128, 24])
    normgT = sb("normgT", [128, 8])
    qkvw = sb("qkvw", [128, 6, 3])
    scw = sb("scw", [128, 2, 3])
    cfw = sb("cfw", [128, 2, 31])
    cvec = sb("cvec", [128, 3, 2])
    rowc = sb("rowc", [128, 784])
    negA = sb("negA", [128, 8])
    wsT32 = sb("wsT32", [128, 4, 128])
    wsT = sb("wsT", [128, 4, 128], BF16)
    bsT = sb("bsT", [128, 2, 128])
    fgB = sb("fgB", [128, 1024])
    xt = sb("xt", [128, 2, 1024])
    xn = sb("xn", [128, 2, 1024], BF16)
    junk = sb("junk", [128, 1024], BF16)
    sm = sb("sm", [128, 64])
    qT = sb("qT", [128, 2, WP], BF16)
    kT = sb("kT", [128, 2, WP], BF16)
    kvtok = sb("kvtok", [128, NT, 512], BF16)
    gq = sb("gq", [128, NT, 8])
    bq = sb("bq", [128, NT, 8])
    nbq = sb("nbq", [128, NT, 8])
    s32 = sb("s32", [128, 2, 2, 64])
    s16 = sb("s16", [128, 2, 2, 64], BF16)
    AE = 8 * 2304 + 5 * 2 * WP
    arena = sb("arena", [128, AE], BF16)
    hT = arena[:, 0:8 * 2304].rearrange("p (k n) -> p k n", k=8)
    WS = [arena[:, 8 * 2304 + i * 2 * WP: 8 * 2304 + (i + 1) * 2 * WP].bitcast(F32) for i in range(5)]
    _off = [0]

    def carve(n_el, dt=BF16):
        a = arena[:, _off[0]:_off[0] + n_el * (2 if dt == F32 else 1)]
        _off[0] += n_el * (2 if dt == F32 else 1)
        assert _off[0] <= 8 * 2304
        return a.bitcast(F32) if dt == F32 else a
    Gb = [carve(128, F32) for _ in range(4)]
    decT = [carve(128, F32) for _ in range(4)]
    decTs = [carve(128, F32) for _ in range(4)]
    Egc = [carve(128, F32) for _ in range(4)]
    XM = [[carve(384) for _ in range(2)] for _ in range(4)]
    ATb = [carve(128) for _ in range(4)]
    negwT = [carve(128) for _ in range(4)]
    qgT = [carve(128) for _ in range(4)]
    kgb = [carve(128) for _ in range(4)]
    kdec = [carve(128) for _ in range(4)]
    kTz = [carve(128) for _ in range(4)]
    tmpS = [carve(64, F32) for _ in range(4)]
    otmp = [carve(64, F32) for _ in range(4)]
    vnew = [[carve(64) for _ in range(2)] for _ in range(4)]
    tsm = carve(32, F32)
    osum = carve(256, F32)
    osq = carve(256, F32)
    yab = carve(256)
    yTt = carve(8 * 128).rearrange("p (k n) -> p k n", k=8)
    gat = carve(2 * 128).rearrange("p (k n) -> p k n", k=2)
    rt = carve(1024, F32)
    of_ap = arena[:, 8 * 2304: 8 * 2304 + 2 * NT * 256].bitcast(F32).rearrange("p (t n) -> p t n", t=NT)
    wa = arena[:, 8 * 2304 + 4 * WP: 8 * 2304 + 4 * WP + 2 * 2 * 1024].bitcast(F32).rearrange("p (i k n) -> p i k n", i=2, k=8)
    Dg = WS[4][:, 0:512]
    ps = [es.enter_context(nc.psum_tensor(f"ps{i}", [128, 512], F32)) for i in range(8)]

    def pk(b, r0=0, r1=4):
        return [("ps", b, r) for r in range(r0, r1)]

    ARK = ["arenaH"]
    WSK = lambda i: [("WS", i)]
    HK = lambda t0, t1: [("hT", t) for t in range(t0, t1)]

    S.add("sp", lambda e: e.dma_start(out=consts[:], in_=consts_d[:]), [], ["consts"], dma=True)
    S.add("sp", lambda e: e.dma_start(out=ccat[:], in_=ccat_d[:]), [], ["ccat"], dma=True)
    S.add("sp", lambda e: e.dma_start(out=fgB[:], in_=fg_d[:]), [], ["fgB"], dma=True)
    S.add("dve", lambda e: e.tensor_copy(out=identb[:], in_=IDENT), ["consts"], ["identb"])
    S.add("act", lambda e: e.activation(out=scT[:], in_=ccat[:], func=AF.Silu), ["ccat"], ["scT"])
    for i in range(5):
        S.add("pool", lambda e, i=i: e.memset(WS[i], 0.0), [], WSK(i))

    def run_rr(gens):
        gens = list(gens)
        while gens:
            for g in list(gens):
                try:
                    next(g)
                except StopIteration:
                    gens.remove(g)

    evac_rr = [0]
    out_keys = []
    bar_n = [0]
    bscr = sb("bscr", [128, 16])
    bdram = dram("bdram", [2, 128, 4], F32, "Internal")

    def full_barrier(extra_writes=()):
        n = bar_n[0]
        bar_n[0] += 1
        S.add("pe", lambda e: e.matmul(ps[0][:, 0:128], identb[:], identb[:], start=True, stop=True), ["identb"], [("bar1", n, "pe")] + pk(0))
        S.add("act", lambda e: e.activation(out=bscr[:, 0:1], in_=bscr[:, 8:9], func=AF.Copy), ["bscr8"], [("bar1", n, "act"), "bscr0"])
        S.add("dve", lambda e: e.tensor_copy(out=bscr[:, 1:2], in_=bscr[:, 8:9]), ["bscr8"], [("bar1", n, "dve"), "bscr1"])
        S.add("sp", lambda e: e.dma_start(out=bdram[0], in_=bscr[:, 8:12]), ["bscr8"], [("bar1", n, "sp"), "bdram0"], dma=True)
        S.add("pool", lambda e: e.memset(bscr[:, 2:3], 0.0), [("bar1", n, k) for k in ("pe", "act", "dve", "sp")],
              [("bar2", n), "bscr2"] + list(extra_writes))
        S.add("pe", lambda e: e.matmul(ps[0][:, 0:128], identb[:], identb[:], start=True, stop=True), [("bar2", n), "identb"], pk(0))
        S.add("act", lambda e: e.activation(out=bscr[:, 3:4], in_=bscr[:, 8:9], func=AF.Copy), [("bar2", n), "bscr8"], ["bscr3"])
        S.add("dve", lambda e: e.tensor_copy(out=bscr[:, 4:5], in_=bscr[:, 8:9]), [("bar2", n), "bscr8"], ["bscr4"])
        S.add("sp", lambda e: e.dma_start(out=bdram[1], in_=bscr[:, 8:12]), [("bar2", n), "bscr8"], ["bdram1"], dma=True)

    S.add("pool", lambda e: e.memset(bscr[:], 0.0), [], ["bscr8", "bscr0", "bscr1", "bscr2", "bscr3", "bscr4"])

    def evac_copy(dst, src, reads, writes, scale=None):
        evac_rr[0] ^= 1
        if evac_rr[0]:
            S.add("act", lambda e: e.activation(out=dst, in_=src, func=AF.Copy, scale=(1.0 if scale is None else scale)),
                  reads, writes)
        else:
            if scale is None:
                S.add("dve", lambda e: e.tensor_copy(out=dst, in_=src), reads, writes)
            else:
                S.add("dve", lambda e: e.tensor_scalar(out=dst, in0=src, scalar1=float(scale), scalar2=None, op0=ALU.mult),
                      reads, writes)

    def tap(name, ap, reads):
        if name in tap_d:
            S.add("pool", lambda e: e.dma_start(out=tap_d[name][:], in_=ap), reads, [("tap", name)], dma=True)

    wch_i = [0]
    psrot = [0]

    def load_wch(l, cc):
        i = wch_i[0] % 3
        wch_i[0] += 1
        S.add("pool", lambda e: e.dma_start(out=wch[:, i, :, :], in_=win_d[l, cc]), [], [("wch", i)], dma=True)
        return i

    GROUPS = [(0, 256)] + [(256 + 512 * g, 512) for g in range(4)]

    def proj_fm(l, cc, dst, dkeys, func=None):
        i = load_wch(l, cc)
        for (t0, n) in GROUPS:
            bnk = psrot[0] % 4
            psrot[0] += 1
            for kc in range(8):
                S.add("pe", lambda e, kc=kc, bnk=bnk, t0=t0, n=n: e.matmul(
                    ps[bnk][:, 0:n], wch[:, i, kc, :], hT[:, kc, t0:t0 + n], start=(kc == 0), stop=(kc == 7)),
                    [("wch", i)] + HK(t0 // 128, (t0 + n) // 128), pk(bnk))
            d = dst[:, wcol(t0):wcol(t0) + n]
            if func is None:
                evac_copy(d, ps[bnk][:, 0:n], pk(bnk), dkeys)
            else:
                S.add("act", lambda e, d=d, bnk=bnk, n=n: e.activation(out=d, in_=ps[bnk][:, 0:n], func=func),
                      pk(bnk), dkeys)

    R0, R1 = 16, 2336

    def zero_mid(i):
        S.add("pool", lambda e: e.memset(WS[i][:, 272:288], 0.0), [], WSK(i))

    def spill(b, c, i):
        for (c0, n0, n) in ((16, 0, 256), (288, 256, 1024), (1312, 1280, 1024)):
            S.add("pool", lambda e, c0=c0, n0=n0, n=n: e.dma_start(out=spill_d[b, c, :, n0:n0 + n], in_=WS[i][:, c0:c0 + n]),
                  WSK(i), [("spill", b, c, n0)], dma=True)

    def tt(eng, out, a, b_, op, reads, writes):
        S.add(eng, lambda e: e.tensor_tensor(out=out, in0=a, in1=b_, op=op), reads, writes)

    def conv_taps(dst, src, wts, ntap, bias, reads, writes):
        half = ntap // 2
        if bias is None:
            S.add("dve", lambda e: e.tensor_scalar(out=dst[:, R0:R1], in0=src[:, R0 - half:R1 - half], scalar1=wts[:, 0:1],
                                                   scalar2=None, op0=ALU.mult), reads, writes)
        else:
            S.add("dve", lambda e: e.tensor_scalar(out=dst[:, R0:R1], in0=src[:, R0 - half:R1 - half], scalar1=wts[:, 0:1],
                                                   scalar2=bias, op0=ALU.mult, op1=ALU.add), reads, writes)
        for k in range(1, ntap):
            S.add("dve", lambda e, k=k: e.scalar_tensor_tensor(out=dst[:, R0:R1], in0=src[:, R0 + k - half:R1 + k - half],
                                                               scalar=wts[:, k:k + 1], in1=dst[:, R0:R1], op0=ALU.mult, op1=ALU.add),
                  reads + writes, writes)

    def rsqrt_inplace(ap, scale, reads_writes):
        S.add("act", lambda e: e.activation(out=ap, in_=ap, func=AF.Sqrt, bias=epsc[:, 0:1], scale=scale), reads_writes + ["epsc"], reads_writes)
        S.add("dve", lambda e: e.reciprocal(out=ap, in_=ap), reads_writes, reads_writes)

    epsc = sb("epsc", [128, 2])
    S.add("pool", lambda e: e.memset(epsc[:, 0:1], EPS), [], ["epsc"])
    S.add("pool", lambda e: e.memset(epsc[:, 1:2], 1.0), ["epsc"], ["epsc"])

    def xsrc(l, b, t):
        if t < 2:
            base = ctx_d if l == 0 else cres_d
            return [(base[b, t * 128:(t + 1) * 128, :], slice(0, 128))]
        base = x_d if l == 0 else xres_d
        tt_ = t - 2
        if l % 2 == 0:
            return [(base[b, tt_ * 128:(tt_ + 1) * 128, :], slice(0, 128))]
        v = base[b].rearrange("(r c) d -> c r d", c=64)
        return [(v[4 * tt_ + i], slice(32 * i, 32 * i + 32)) for i in range(4)]

    def xdst(l, b, t, last):
        if t < 2:
            return [(cres_d[b, t * 128:(t + 1) * 128, :], slice(0, 128))]
        base = y_d if last else xres_d
        tt_ = t - 2
        if l % 2 == 0:
            return [(base[b, tt_ * 128:(tt_ + 1) * 128, :], slice(0, 128))]
        v = base[b].rearrange("(r c) d -> c r d", c=64)
        return [(v[4 * tt_ + i], slice(32 * i, 32 * i + 32)) for i in range(4)]

    def xkeys(b, t):
        return [("cr", b, t)] if t < 2 else [("xr", b, tt_, i_) for tt_ in range(16) for i_ in range(4)]

    def XTK(slot):
        return [("xt", slot, i_) for i_ in range(4)]

    def load_xt(l, b, t, slot):
        srcs = xsrc(l, b, t)
        for i_, (src, psl) in enumerate(srcs):
            S.add("sp", lambda e, src=src, psl=psl: e.dma_start(out=xt[psl, slot, :], in_=src),
                  (xkeys(b, t) if l > 0 else []), (XTK(slot) if len(srcs) == 1 else [("xt", slot, i_)]), dma=True)

    cur_l = [0]

    def chk(level):
        if stop == level and cur_l[0] == STOPL:
            raise _Stop()

    def layer_prologue(l, last):
        chk(10)
        for dst, src, key in ((badaT, badaT_d[l], "badaT"), (normgT, normgT_d[l], "normgT"), (qkvw, qkvw_d[l], "qkvw"),
                              (scw, scw_d[l], "scw"), (cfw, cfw_d[l], "cfw"), (cvec, cvec_d[l], "cvec"),
                              (rowc, rowc_d[l], "rowc"), (wsT32, wsT_d[l], "wsT32"), (bsT, bsT_d[l], "bsT")):
            S.add("sp", lambda e, dst=dst, src=src: e.dma_start(out=dst[:], in_=src), [], [key], dma=True)
        for h2 in range(8):
            S.add("pool", lambda e, h2=h2: e.dma_start(out=wout[:, h2, :], in_=wout_d[l, :, h2, :]),
                  [], [("wout", h2)], dma=True)
        S.add("dve", lambda e: e.tensor_copy(out=wsT[:], in_=wsT32[:]), ["wsT32"], ["wsT"])
        S.add("act", lambda e: e.activation(out=negA[:], in_=rowc[:, 768:776], func=AF.Exp), ["rowc"], ["negA"])
        S.add("dve", lambda e: e.tensor_scalar(out=negA[:], in0=negA[:], scalar1=-1.0, scalar2=None, op0=ALU.mult), ["negA"], ["negA"])
        chk(11)
        for cc in range(24):
            wi = cc % 2
            S.add("sp", lambda e, cc=cc, wi=wi: e.dma_start(out=wa[:, wi, :, :], in_=wada_d[l, cc]),
                  [], [("wa", wi)] + WSK(2) + WSK(3), dma=True)
            reg = 6 + cc % 2
            for kc in range(8):
                S.add("pe", lambda e, kc=kc, wi=wi, reg=reg: e.matmul(ps[reg][:, 0:3], wa[:, wi, kc, :], scT[:, kc, :],
                                                                    start=(kc == 0), stop=(kc == 7)),
                      [("wa", wi), "scT"] + WSK(2) + WSK(3), [("ps", reg, 0)])
            S.add("dve", lambda e, cc=cc, reg=reg: e.tensor_scalar(out=modT[:, cc, :], in0=ps[reg][:, 0:3],
                                                                   scalar1=badaT[:, cc:cc + 1], scalar2=None, op0=ALU.add),
                  [("ps", reg, 0), "badaT"], [("modT", cc)])
        for kc in range(8):
            S.add("dve", lambda e, kc=kc: e.tensor_scalar(out=sc1T[:, kc, :], in0=modT[:, 8 + kc, :], scalar1=1.0,
                                                          scalar2=normgT[:, kc:kc + 1], op0=ALU.add, op1=ALU.mult),
                  [("modT", 8 + kc), "normgT"], [("sc1T", kc)])
        for r in range(3):
            for hf in range(2):
                for j in range(4):
                    kc = hf * 4 + j
                    S.add("dve", lambda e, j=j, kc=kc, r=r: e.tensor_scalar(out=Dg[:, j * 128:(j + 1) * 128], in0=IDENT,
                                                                           scalar1=modT[:, 16 + kc, r:r + 1], scalar2=None, op0=ALU.mult),
                          [("modT", 16 + kc), "consts"], WSK(4))
                bnk = 4 + (r * 2 + hf) % 2
                S.add("pe", lambda e, bnk=bnk: e.matmul(ps[bnk][:, :], ONES, Dg, start=True, stop=True), WSK(4) + ["consts"], pk(bnk))
                evac_copy(gateB[:, r, hf * 512:(hf + 1) * 512], ps[bnk][:, :], pk(bnk), [("gateB", r)])

        if l == TAPL:
            tap("modT", modT[:].rearrange("p a b -> p (a b)"), [("modT", i_) for i_ in range(24)])
            tap("sc1T", sc1T[:].rearrange("p a b -> p (a b)"), [("sc1T", i_) for i_ in range(8)])
            tap("gateB", gateB[:, 2, :], [("gateB", 2)])

    def batch_body(l, b, last):
        for t in range(NT):
            slot = t % 2
            r = 2 if t < 2 else b
            load_xt(l, b, t, slot)
            xk = XTK(slot)
            if l == TAPL and b == 0 and t == 0:
                tap("xt0", xt[:, 0, :], xk)
            if l == TAPL and b == 0 and t == 2:
                tap("xt2", xt[:, 0, :], xk)
            S.add("act", lambda e, slot=slot: e.activation(out=junk[:], in_=xt[:, slot, :], func=AF.Square, accum_out=sm[:, slot:slot + 1]),
                  xk, ["junk", ("sm", slot)], na=True)
            rsqrt_inplace(sm[:, slot:slot + 1], 1.0 / 1024, [("sm", slot)])
            S.add("act", lambda e, slot=slot: e.activation(out=xn[:, slot, :], in_=xt[:, slot, :], func=AF.Copy, scale=sm[:, slot:slot + 1]),
                  xk + [("sm", slot)], [("xn", slot)])
            for hf in range(2):
                bnk = 4 + hf
                for j in range(4):
                    kc = hf * 4 + j
                    S.add("pe", lambda e, j=j, kc=kc, bnk=bnk, slot=slot: e.matmul(
                        ps[bnk][:, j * 128:(j + 1) * 128], xn[:, slot, kc * 128:(kc + 1) * 128], identb[:], start=True, stop=True),
                        [("xn", slot), "identb"], [("ps", bnk, j)])
                for j in range(4):
                    kc = hf * 4 + j
                    o_ = hT[:, kc, t * 128:(t + 1) * 128]
                    i_ = ps[bnk][:, j * 128:(j + 1) * 128]
                    rk = [("ps", bnk, j), ("sc1T", kc), ("modT", kc)]
                    if j % 2 == 0:
                        S.add("act", lambda e, o_=o_, i_=i_, kc=kc, r=r: e.activation(out=o_, in_=i_, func=AF.Identity,
                                                                                  scale=sc1T[:, kc, r:r + 1], bias=modT[:, kc, r:r + 1]),
                              rk, [("hT", t)])
                    else:
                        S.add("dve", lambda e, o_=o_, i_=i_, kc=kc, r=r: e.tensor_scalar(out=o_, in0=i_, scalar1=sc1T[:, kc, r:r + 1],
                                                                                      scalar2=modT[:, kc, r:r + 1], op0=ALU.mult, op1=ALU.add),
                              rk, [("hT", t)])

        if l == TAPL and b == 0:
            for i_ in range(2):
                tap(f"hT{i_}", hT[:, 0, i_ * 1152:(i_ + 1) * 1152], HK(0, NT))
        chk(2)
        for i_ in range(5):
            for (a_, b_) in ((0, 16), (272, 288), (2336, 2352)):
                S.add("pool", lambda e, i_=i_, a_=a_, b_=b_: e.memset(WS[i_][:, a_:b_], 0.0), [], WSK(i_))
        for j in range(2):
            proj_fm(l, 21 + j, WS[j], WSK(j), AF.Silu)
            spill(b, j, j)
        for j in range(2):
            zero_mid(0), zero_mid(1)
            proj_fm(l, 9 + j, WS[0], WSK(0))
            proj_fm(l, 11 + j, WS[1], WSK(1))
            tt("pool", WS[0][:, R0:R1], WS[0][:, R0:R1], WS[1][:, R0:R1], ALU.mult, WSK(0) + WSK(1), WSK(0))
            conv_taps(WS[1], WS[0], scw[:, j, :], 3, None, WSK(0) + ["scw"], WSK(1))
            proj_fm(l, 7 + j, WS[2], WSK(2))
            proj_fm(l, 23 + j, WS[3], WSK(3), AF.Silu)
            tt("pool", WS[2][:, R0:R1], WS[2][:, R0:R1], WS[1][:, R0:R1], ALU.mult, WSK(2) + WSK(1), WSK(2))
            tt("dve", WS[2][:, R0:R1], WS[2][:, R0:R1], WS[3][:, R0:R1], ALU.mult, WSK(2) + WSK(3), WSK(2))
            spill(b, 2 + j, 2)
        for j in range(2):
            zero_mid(0)
            proj_fm(l, 13 + j, WS[0], WSK(0))
            proj_fm(l, 15 + j, WS[1], WSK(1), AF.Sigmoid)
            tt("pool", WS[0][:, R0:R1], WS[0][:, R0:R1], WS[1][:, R0:R1], ALU.mult, WSK(0) + WSK(1), WSK(0))
            conv_taps(WS[2 + j], WS[0], cfw[:, j, :], 31, cvec[:, 0, j:j + 1], WSK(0) + ["cfw", "cvec"], WSK(2 + j))
        for (t0, n) in GROUPS:
            c0 = wcol(t0)
            sq = [WS[0][:, 0:512], WS[0][:, 512:1024]]
            mg, vg = WS[0][:, 1024:1536], WS[0][:, 1536:2048]
            for j in range(2):
                S.add("act", lambda e, j=j, c0=c0, n=n: e.activation(out=sq[j][:, 0:n], in_=WS[2 + j][:, c0:c0 + n], func=AF.Square),
                      WSK(2 + j), WSK(0))
            for j in range(2):
                S.add("pe", lambda e, j=j, c0=c0, n=n: e.matmul(ps[4][:, 0:n], ONES, WS[2 + j][:, c0:c0 + n], start=(j == 0), stop=(j == 1)),
                      WSK(2 + j) + ["consts"], pk(4))
            for j in range(2):
                S.add("pe", lambda e, j=j, n=n: e.matmul(ps[5][:, 0:n], ONES, sq[j][:, 0:n], start=(j == 0), stop=(j == 1)),
                      WSK(0) + ["consts"], pk(5))
            S.add("act", lambda e, n=n: e.activation(out=mg[:, 0:n], in_=ps[4][:, 0:n], func=AF.Copy, scale=1.0 / 256), pk(4), WSK(0))
            S.add("act", lambda e, n=n: e.activation(out=vg[:, 0:n], in_=ps[4][:, 0:n], func=AF.Square, scale=1.0 / 256), pk(4), WSK(0))
            S.add("dve", lambda e, n=n: e.scalar_tensor_tensor(out=vg[:, 0:n], in0=ps[5][:, 0:n], scalar=1.0 / 256, in1=vg[:, 0:n],
                                                              op0=ALU.mult, op1=ALU.subtract), pk(5) + WSK(0), WSK(0))
            rsqrt_inplace(vg[:, 0:n], 1.0, WSK(0))
            for j in range(2):
                z = WS[2 + j][:, c0:c0 + n]
                tt("dve", z, z, mg[:, 0:n], ALU.subtract, WSK(2 + j) + WSK(0), WSK(2 + j))
                tt("pool", z, z, vg[:, 0:n], ALU.mult, WSK(2 + j) + WSK(0), WSK(2 + j))
        for j in range(2):
            z = WS[2 + j][:, R0:R1]
            S.add("act", lambda e, z=z, j=j: e.activation(out=z, in_=z, func=AF.Silu, scale=cvec[:, 1, j:j + 1], bias=cvec[:, 2, j:j + 1]),
                  WSK(2 + j) + ["cvec"], WSK(2 + j))
            proj_fm(l, 25 + j, WS[1], WSK(1), AF.Silu)
            tt("dve", z, z, WS[1][:, R0:R1], ALU.mult, WSK(2 + j) + WSK(1), WSK(2 + j))
            spill(b, 4 + j, 2 + j)
        wiA = load_wch(l, 19)
        wiB = load_wch(l, 20)
        vt32 = WS[0][:, 0:256]
        vnz = WS[1][:, 0:256].bitcast(BF16)
        vnz3 = vnz.rearrange("p (j g x) -> p j g x", j=2, g=2)
        vt3 = vt32.rearrange("p (j g d) -> p j g d", j=2, g=2)
        lnb3 = rowc[:, 256:512].rearrange("p (j g d) -> p j g d", j=2, g=2)
        S.add("pool", lambda e: e.memset(WS[1][:, 0:256], 0.0), [], WSK(1))
        for t in range(NT):
            for jj, wi in ((0, wiA), (1, wiB)):
                for kc in range(8):
                    S.add("pe", lambda e, jj=jj, wi=wi, kc=kc, t=t: e.matmul(ps[4][:, jj * 128:(jj + 1) * 128], hT[:, kc, t * 128:(t + 1) * 128],
                                                                            wch[:, wi, kc, :], start=(kc == 0), stop=(kc == 7)),
                          [("wch", wi), ("hT", t)], pk(4, jj, jj + 1))
            S.add("act", lambda e: e.activation(out=vt32, in_=ps[4][:, 0:256], func=AF.Copy, accum_out=sm[:, 8:9]), pk(4, 0, 2), WSK(0) + [("sm", 8)], na=True)
            S.add("act", lambda e: e.activation(out=junk[:, 0:256], in_=ps[4][:, 0:256], func=AF.Square, accum_out=sm[:, 9:10]), pk(4, 0, 2), ["junk", ("sm", 9)], na=True)
            S.add("dve", lambda e: e.tensor_scalar(out=sm[:, 8:9], in0=sm[:, 8:9], scalar1=1.0 / 256, scalar2=None, op0=ALU.mult), [("sm", 8)], [("sm", 8)])
            S.add("dve", lambda e: e.tensor_tensor(out=sm[:, 10:11], in0=sm[:, 8:9], in1=sm[:, 8:9], op=ALU.mult), [("sm", 8)], [("sm", 10)])
            S.add("dve", lambda e: e.scalar_tensor_tensor(out=sm[:, 9:10], in0=sm[:, 9:10], scalar=1.0 / 256, in1=sm[:, 10:11], op0=ALU.mult, op1=ALU.subtract),
                  [("sm", 9), ("sm", 10)], [("sm", 9)])
            rsqrt_inplace(sm[:, 9:10], 1.0, [("sm", 9)])
            S.add("dve", lambda e: e.tensor_scalar(out=vt32, in0=vt32, scalar1=sm[:, 8:9], scalar2=sm[:, 9:10], op0=ALU.subtract, op1=ALU.mult),
                  WSK(0) + [("sm", 8), ("sm", 9)], WSK(0))
            tt("pool", vt32, vt32, rowc[:, 0:256], ALU.mult, WSK(0) + ["rowc"], WSK(0))
            for g2 in range(2):
                tt("dve", vnz3[:, :, g2, g2 * 64:(g2 + 1) * 64], vt3[:, :, g2, :], lnb3[:, :, g2, :], ALU.add, WSK(0) + ["rowc"], WSK(1))
            for j in range(2):
                for g2 in range(2):
                    g = 2 * j + g2
                    S.add("pe", lambda e, j=j, g2=g2, g=g: e.matmul(ps[5][:, j * 128:(j + 1) * 128], vnz3[:, j, g2, :],
                                                                   wsT[:, g, :], start=(g2 == 0), stop=(g2 == 1)),
                          WSK(1) + ["wsT"], pk(5, j, j + 1))
            for j in range(2):
                c0 = wcol(t * 128)
                tt("dve", WS[2 + j][:, c0:c0 + 128], ps[5][:, j * 128:(j + 1) * 128], bsT[:, j, :], ALU.add, pk(5, j, j + 1) + ["bsT"], WSK(2 + j))
        for j in range(2):
            proj_fm(l, 17 + j, WS[0], WSK(0))
            proj_fm(l, 27 + j, WS[1], WSK(1), AF.Silu)
            tt("pool", WS[0][:, R0:R1], WS[0][:, R0:R1], WS[2 + j][:, R0:R1], ALU.mult, WSK(0) + WSK(2 + j), WSK(0))
            tt("dve", WS[0][:, R0:R1], WS[0][:, R0:R1], WS[1][:, R0:R1], ALU.mult, WSK(0) + WSK(1), WSK(0))
            spill(b, 6 + j, 0)

        chk(3)
        for cc in range(6):
            src = WS[cc % 2]
            zero_mid(cc % 2)
            proj_fm(l, cc, src, WSK(cc % 2))
            conv_taps(WS[2], src, qkvw[:, cc, :], 3, None, WSK(cc % 2) + ["qkvw"], WSK(2))
            S.add("act", lambda e: e.activation(out=WS[2][:, R0:R1], in_=WS[2][:, R0:R1], func=AF.Silu), WSK(2), WSK(2))
            if cc < 4:
                dstT = qT if cc < 2 else kT
                S.add("act", lambda e: e.activation(out=WS[3][:, R0:R1], in_=WS[2][:, R0:R1], func=AF.Square), WSK(2), WSK(3))
                for (t0, n) in GROUPS:
                    c0 = wcol(t0)
                    S.add("pe", lambda e, c0=c0, n=n: e.matmul(ps[4][:, 0:n], BO, WS[3][:, c0:c0 + n], start=True, stop=True),
                          WSK(3) + ["consts"], pk(4))
                    rn = WS[4][:, 0:n]
                    S.add("act", lambda e, n=n, rn=rn: e.activation(out=rn, in_=ps[4][:, 0:n], func=AF.Sqrt, bias=epsc[:, 0:1], scale=1.0),
                          pk(4) + ["epsc"], WSK(4))
                    S.add("dve", lambda e, rn=rn: e.reciprocal(out=rn, in_=rn), WSK(4), WSK(4))
                    S.add("dve", lambda e, c0=c0, n=n, rn=rn, dstT=dstT, cc=cc: e.scalar_tensor_tensor(
                        out=dstT[:, cc % 2, c0:c0 + n], in0=WS[2][:, c0:c0 + n], scalar=(0.125 if cc < 2 else 1.0), in1=rn,
                        op0=ALU.mult, op1=ALU.mult), WSK(2) + WSK(4), [("qkT", cc)])
            if cc >= 2:
                for t in range(NT):
                    c0 = wcol(t * 128)
                    bnk = 5 + t % 2
                    if cc < 4:
                        S.add("pe", lambda e, c0=c0, bnk=bnk, cc=cc: e.matmul(ps[bnk][:, 0:128], kT[:, cc % 2, c0:c0 + 128], identb[:], start=True, stop=True),
                              [("qkT", cc), "identb"], pk(bnk, 0, 1))
                    else:
                        S.add("pe", lambda e, c0=c0, bnk=bnk: e.matmul(ps[bnk][:, 0:128], WS[2][:, c0:c0 + 128], IDENT, start=True, stop=True),
                              WSK(2) + ["consts"], pk(bnk, 0, 1))
                    evac_copy(kvtok[:, t, (cc - 2) * 128:(cc - 1) * 128], ps[bnk][:, 0:128], pk(bnk, 0, 1), [("kvtok", t)])
        wi = load_wch(l, 6)
        for t in range(NT):
            bnk = 5 + t % 2
            for kc in range(8):
                S.add("pe", lambda e, kc=kc, t=t, bnk=bnk: e.matmul(ps[bnk][:, 128:144], hT[:, kc, t * 128:(t + 1) * 128], wch[:, wi, kc, 0:16],
                                                                   start=(kc == 0), stop=(kc == 7)),
                      [("wch", wi), ("hT", t)], pk(bnk, 1, 2))
            tmp = sm[:, 16:24]
            tt("dve", tmp, ps[bnk][:, 128:136], rowc[:, 776:784], ALU.add, pk(bnk, 1, 2) + ["rowc"], [("sm", 16)])
            S.add("act", lambda e, tmp=tmp: e.activation(out=tmp, in_=tmp, func=AF.Exp), [("sm", 16)], [("sm", 16)])
            S.add("act", lambda e, tmp=tmp: e.activation(out=tmp, in_=tmp, func=AF.Ln, bias=epsc[:, 1:2], scale=1.0), [("sm", 16), "epsc"], [("sm", 16)])
            tt("dve", gq[:, t, :], tmp, negA[:], ALU.mult, [("sm", 16), "negA"], [("gb", t)])
            S.add("act", lambda e, t=t, bnk=bnk: e.activation(out=bq[:, t, :], in_=ps[bnk][:, 136:144], func=AF.Sigmoid), pk(bnk, 1, 2), [("gb", t)])
            S.add("dve", lambda e, t=t: e.tensor_scalar(out=nbq[:, t, :], in0=bq[:, t, :], scalar1=-1.0, scalar2=None, op0=ALU.mult), [("gb", t)], [("gb", t)])

        chk(4)
        full_barrier()
        for h_ in range(4):
            S.add("pool", lambda e, h_=h_: e.memset(negwT[h_], 0.0), [], [("negwT", h_)])
            S.add("pool", lambda e, h_=h_: e.memset(qgT[h_], 0.0), [], [("qgT", h_)])
            S.add("pool", lambda e, h_=h_: e.memset(kgb[h_], 0.0), [], [("kg", h_)])
            S.add("pool", lambda e, h_=h_: e.memset(kdec[h_], 0.0), [], [("kdec", h_)])
            S.add("pool", lambda e, h_=h_: e.memset(kTz[h_], 0.0), [], [("kTz", h_)])
            for c_ in range(2):
                S.add("pool", lambda e, h_=h_, c_=c_: e.memset(vnew[h_][c_], 0.0), [], [("vnew", h_, c_)])
        chk(5)
        S.add("pool", lambda e: e.memset(s32[:], 0.0), [], [("s", d, h) for d in range(2) for h in range(4)])
        S.add("pool", lambda e: e.memset(s16[:], 0.0), [], [("s16", d, h) for d in range(2) for h in range(4)])
        allH = HK(0, NT)

        def prep_tile(t, d):
            c0 = wcol(t * 128)
            pg = ps[1][:, 384:392]
            S.add("pe", lambda e: e.matmul(ps[1][:, 384:388], TRI[d], gq[:, t, 4 * d:4 * d + 4], start=True, stop=True), [("gb", t), "consts"], [("ps", 1, 3)])
            S.add("pe", lambda e: e.matmul(ps[1][:, 388:392], BO, gq[:, t, 4 * d:4 * d + 4], start=True, stop=True), [("gb", t), "consts"], [("ps", 1, 3)])
            TK = ["tsm"]
            S.add("dve", lambda e: e.tensor_scalar(out=tsm[:, 0:4], in0=ps[1][:, 384:388], scalar1=-1.0, scalar2=None, op0=ALU.mult),
                  [("ps", 1, 3)], TK)
            S.add("act", lambda e: e.activation(out=tsm[:, 4:8], in_=ps[1][:, 384:388], func=AF.Exp), [("ps", 1, 3)], TK)
            tt("dve", tsm[:, 12:16], ps[1][:, 388:392], tsm[:, 0:4], ALU.add, [("ps", 1, 3)] + TK, TK)
            S.add("act", lambda e: e.activation(out=tsm[:, 8:12], in_=tsm[:, 12:16], func=AF.Exp), TK, TK)
            def head_gen(h):
                pb, hp, col = 64 * (h % 2), h // 2, 4 * d + h
                A, Bk = ps[2 * h], ps[2 * h + 1]
                kTh = kT[pb:pb + 64, hp, c0:c0 + 128]
                qTh = qT[pb:pb + 64, hp, c0:c0 + 128]
                hk = [("H", h)]
                S.add("pool", lambda e, h=h, pb=pb, kTh=kTh: e.tensor_copy(out=kTz[h][pb:pb + 64, :], in_=kTh), [("qkT", 2), ("qkT", 3)], [("kTz", h)])
                kTf = kT[:, hp, c0:c0 + 128]
                qTf = qT[:, hp, c0:c0 + 128]
                S.add("pe", lambda e, A=A, h=h, kTf=kTf: e.matmul(A[:, 0:128], kTz[h], kTf, start=True, stop=True), [("kTz", h), ("qkT", 2), ("qkT", 3)], [("ps", 2 * h, 0)])
                S.add("pe", lambda e, A=A, h=h, qTf=qTf: e.matmul(A[:, 128:256], kTz[h], qTf, start=True, stop=True),
                      [("kTz", h), ("qkT", 0), ("qkT", 1)], [("ps", 2 * h, 1)])
                S.add("pool", lambda e, h=h, col=col: e.tensor_scalar(out=Gb[h], in0=TRI[d], scalar1=gq[:, t, col:col + 1], scalar2=0.0, op0=ALU.mult, op1=ALU.add),
                      [("gb", t), "consts"], [("G", h)])
                S.add("pe", lambda e, A=A, h=h: e.matmul(A[:, 256:384], ONES, Gb[h], start=True, stop=False), [("G", h), "consts"], [("ps", 2 * h, 2)])
                S.add("pe", lambda e, A=A: e.matmul(A[:, 256:384], IDENT, NEGM[d], start=False, stop=True), ["consts"], [("ps", 2 * h, 2)])
                S.add("pe", lambda e, A=A, h=h: e.matmul(A[:, 384:512], ONES, Gb[h], start=True, stop=True), [("G", h), "consts"], [("ps", 2 * h, 3)])
                yield
                S.add("act", lambda e, A=A, h=h: e.activation(out=decT[h], in_=A[:, 256:384], func=AF.Exp, bias=tsm[:, h:h + 1], scale=1.0),
                      [("ps", 2 * h, 2)] + TK, [("decT", h)])
                S.add("act", lambda e, A=A, h=h: e.activation(out=Egc[h], in_=A[:, 384:512], func=AF.Exp), [("ps", 2 * h, 3)], [("Egc", h)])
                tt("pool", decTs[h], decT[h], SMM[d], ALU.mult, [("decT", h), "consts"], [("decTs", h)])
                X0 = XM[h][0]
                S.add("dve", lambda e, A=A, X0=X0, h=h, col=col: e.scalar_tensor_tensor(out=X0[:, 128:256], in0=A[:, 0:128], scalar=nbq[:, t, col:col + 1],
                                                                                   in1=decTs[h], op0=ALU.mult, op1=ALU.mult),
                      [("ps", 2 * h, 0), ("gb", t), ("decTs", h)], [("XM", h, 0)])
                tt("dve", ATb[h], A[:, 128:256], decT[h], ALU.mult, [("ps", 2 * h, 1), ("decT", h)], [("AT", h)])
                yield
                S.add("pe", lambda e, A=A, X0=X0: e.matmul(A[:, 0:128], X0[:, 128:256], identb[:], start=True, stop=True), [("XM", h, 0), "identb"], [("ps", 2 * h, 0)])
                S.add("act", lambda e, A=A, X0=X0: e.activation(out=X0[:, 256:384], in_=A[:, 0:128], func=AF.Copy), [("ps", 2 * h, 0)], [("XM", h, 0)])
                S.add("pool", lambda e, X0=X0: e.tensor_copy(out=X0[:, 0:128], in_=identb[:]), ["identb"], [("XM", h, 0)])
                yield
                for it in range(6):
                    cur, nxt = XM[h][it % 2], XM[h][(it + 1) % 2]
                    ck, nk = [("XM", h, it % 2)], [("XM", h, (it + 1) % 2)]
                    S.add("pe", lambda e, Bk=Bk, cur=cur, it=it: e.matmul(Bk[:, 0:(256 if it < 5 else 128)], cur[:, 256:384], cur[:, 0:(256 if it < 5 else 128)],
                                                                         start=True, stop=True), ck, [("ps", 2 * h + 1, 0), ("ps", 2 * h + 1, 1)])
                    if it < 5:
                        S.add("pe", lambda e, Bk=Bk, cur=cur: e.matmul(Bk[:, 256:384], cur[:, 128:256], cur[:, 256:384], start=True, stop=True),
                              ck, [("ps", 2 * h + 1, 2)])
                        S.add("act", lambda e, Bk=Bk, nxt=nxt: e.activation(out=nxt[:, 128:384], in_=Bk[:, 128:384], func=AF.Copy),
                              [("ps", 2 * h + 1, 1), ("ps", 2 * h + 1, 2)], nk)
                    tt("dve", nxt[:, 0:128], Bk[:, 0:128], cur[:, 0:128], ALU.add, [("ps", 2 * h + 1, 0)] + ck, nk)
                    yield
                Xf = XM[h][0][:, 0:128]
                ktok = kvtok[:, t, h * 64:(h + 1) * 64]
                S.add("pool", lambda e, h=h, ktok=ktok: e.tensor_scalar(out=kgb[h][:, 64 * (h % 2):64 * (h % 2) + 64], in0=ktok, scalar1=tsm[:, 4 + h:5 + h], scalar2=0.0, op0=ALU.mult, op1=ALU.add),
                      [("kvtok", t)] + TK, [("kg", h)])
                S.add("pool", lambda e, h=h, ktok=ktok: e.tensor_scalar(out=kdec[h][:, 64 * (h % 2):64 * (h % 2) + 64], in0=ktok, scalar1=tsm[:, 8 + h:9 + h], scalar2=0.0, op0=ALU.mult, op1=ALU.add),
                      [("kvtok", t)] + TK, [("kdec", h)])
                S.add("pe", lambda e, A=A, h=h, Xf=Xf, pb=pb: e.matmul(A[:, 0:128], kgb[h], Xf, start=True, stop=True),
                      [("kg", h), ("XM", h, 0)], [("ps", 2 * h, 0)])
                yield
                S.add("act", lambda e, A=A, h=h, pb=pb: e.activation(out=negwT[h][pb:pb + 64, :], in_=A[pb:pb + 64, 0:128], func=AF.Copy, scale=-1.0),
                      [("ps", 2 * h, 0)], [("negwT", h)])
                tt("pool", qgT[h][pb:pb + 64, :], qTh, Egc[h][pb:pb + 64, :], ALU.mult, [("qkT", 0), ("qkT", 1), ("Egc", h)], [("qgT", h)])
                yield
            run_rr([head_gen(h) for h in range(4)])

        def scan_chunk(t, d, c, h, fin):
            pb, hp, col = 64 * (h % 2), h // 2, 4 * d + h
            A = ps[2 * h]
            r = slice(64 * c, 64 * c + 64)
            Xf = XM[h][0][:, 0:128]
            sk, s16k = [("s", d, h)], [("s16", d, h)]
            vtok = kvtok[r, t, 256 + h * 64:256 + (h + 1) * 64]
            s16h = s16[pb:pb + 64, d, hp, :]
            s16f = s16[:, d, hp, :]
            s16fk = [("s16", d, 2 * hp), ("s16", d, 2 * hp + 1)]
            s32h = s32[pb:pb + 64, d, hp, :]
            vn = vnew[h][c]
            vk = [("vnew", h, c)]
            PV, PO, PS_, PSf = A[:, 128:192], A[:, 192:256], A[pb:pb + 64, 256:320], A[:, 256:320]
            S.add("pe", lambda e: e.matmul(PV, Xf, kvtok[:, t, 256 + h * 64:256 + (h + 1) * 64], start=True, stop=False), [("XM", h, 0), ("kvtok", t)], [("ps", 2 * h, 1)])
            S.add("pe", lambda e: e.matmul(PV, negwT[h], s16f, start=False, stop=True), [("negwT", h)] + s16fk, [("ps", 2 * h, 1)])
            yield
            S.add("act", lambda e: e.activation(out=vn[r, :], in_=A[r, 128:192], func=AF.Copy, scale=bq[r, t, col:col + 1]),
                  [("ps", 2 * h, 1), ("gb", t)], vk)
            yield
            S.add("pe", lambda e: e.matmul(PO, qgT[h], s16f, start=True, stop=False), [("qgT", h)] + s16fk, [("ps", 2 * h, 1)])
            S.add("pe", lambda e: e.matmul(PO, ATb[h], vn, start=False, stop=True), [("AT", h)] + vk, [("ps", 2 * h, 1)])
            S.add("pe", lambda e: e.matmul(PSf, kdec[h], vn, start=True, stop=True), [("kdec", h)] + vk, [("ps", 2 * h, 2)])
            yield
            if not fin:
                S.add("act", lambda e: e.activation(out=of_ap[r, t, h * 64:(h + 1) * 64], in_=A[r, 192:256], func=AF.Copy),
                      [("ps", 2 * h, 1)], [("of", t)])
            else:
                S.add("act", lambda e: e.activation(out=otmp[h][r, :], in_=A[r, 192:256], func=AF.Copy), [("ps", 2 * h, 1)], [("otmp", h)])
                tt("dve", osum[r, h * 64:(h + 1) * 64], otmp[h][r, :], of_ap[r, t, h * 64:(h + 1) * 64], ALU.add,
                   [("otmp", h), ("of", t)], [("osum", h)])
            clast = 64 * c + 63 if d == 0 else 64 * c
            S.add("dve", lambda e: e.tensor_scalar(out=s32h, in0=s32h, scalar1=Egc[h][pb:pb + 64, clast:clast + 1], scalar2=None, op0=ALU.mult),
                  [("Egc", h)] + sk, sk)
            S.add("act", lambda e: e.activation(out=tmpS[h][pb:pb + 64, :], in_=PS_, func=AF.Copy), [("ps", 2 * h, 2)], [("tmpS", h)])
            S.add("dve", lambda e: e.tensor_tensor(out=s32h, in0=tmpS[h][pb:pb + 64, :], in1=s32h, op=ALU.add), [("tmpS", h)] + sk, sk)
            S.add("act", lambda e: e.activation(out=s16h, in_=s32h, func=AF.Copy), sk, s16k)
            yield

        def finalize(t):
            r = 2 if t < 2 else b
            ok = [("osum", h) for h in range(4)]
            S.add("act", lambda e: e.activation(out=osq, in_=osum, func=AF.Square), ok, ["osq"])
            S.add("dve", lambda e: e.tensor_reduce(out=sm[:, 32:36], in_=osq.rearrange("p (h d) -> p h d", h=4), axis=AX.X, op=ALU.add),
                  ["osq"], [("sm", 32)])
            rsqrt_inplace(sm[:, 32:36], 1.0 / 64, [("sm", 32)])
            for h in range(4):
                S.add("dve", lambda e, h=h: e.scalar_tensor_tensor(out=yab[:, h * 64:(h + 1) * 64], in0=osum[:, h * 64:(h + 1) * 64], scalar=sm[:, 32 + h:33 + h],
                                                                  in1=rowc[:, 512 + h * 64:512 + (h + 1) * 64], op0=ALU.mult, op1=ALU.mult),
                      [("osum", h), ("sm", 32), "rowc"], ["yab"])
            n0 = t * 128
            part = 0 if t < 2 else (256 if t < 10 else 1280)
            S.add("sp", lambda e: e.dma_start(out=yTt[:, 2:8, :], in_=spill_d[b, 2:8, :, n0:n0 + 128].rearrange("c p n -> p c n")),
                  [("spill", b, c, part) for c in range(2, 8)], ["yTt"], dma=True)
            S.add("sp", lambda e: e.dma_start(out=gat[:, :, :], in_=spill_d[b, 0:2, :, n0:n0 + 128].rearrange("c p n -> p c n")),
                  [("spill", b, c, part) for c in range(0, 2)], ["gat"], dma=True)
            for j in range(2):
                S.add("pe", lambda e, j=j: e.matmul(ps[3][:, j * 128:(j + 1) * 128], yab[:, j * 128:(j + 1) * 128], identb[:], start=True, stop=True),
                      ["yab", "identb"], [("ps", 3, j)])
                tt("dve", yTt[:, j, :], ps[3][:, j * 128:(j + 1) * 128], gat[:, j, :], ALU.mult, [("ps", 3, j), "gat"], ["yTt"])
            for nh in range(2):
                for kc in range(8):
                    S.add("pe", lambda e, nh=nh, kc=kc: e.matmul(ps[5 + 2 * nh][:, :], yTt[:, kc, :], wout[:, kc, nh * 512:(nh + 1) * 512],
                                                                start=(kc == 0), stop=(kc == 7)), ["yTt", ("wout", kc)], pk(5 + 2 * nh))
            load_xt(l, b, t, 0)
            for nh in range(2):
                tt("dve", rt[:, nh * 512:(nh + 1) * 512], ps[5 + 2 * nh][:, :], gateB[:, r, nh * 512:(nh + 1) * 512], ALU.mult,
                   pk(5 + 2 * nh) + [("gateB", r)], ["rt"])
            tt("pool", rt[:, :], rt[:, :], xt[:, 0, :], ALU.add, ["rt"] + XTK(0), ["rt"])
            if last:
                S.add("act", lambda e: e.activation(out=junk[:], in_=rt[:, :], func=AF.Square, accum_out=sm[:, 40:41]), ["rt"], ["junk", ("sm", 40)], na=True)
                rsqrt_inplace(sm[:, 40:41], 1.0 / 1024, [("sm", 40)])
                S.add("dve", lambda e: e.scalar_tensor_tensor(out=rt[:, :], in0=rt[:, :], scalar=sm[:, 40:41], in1=fgB[:], op0=ALU.mult, op1=ALU.mult),
                      ["rt", ("sm", 40), "fgB"], ["rt"])
            dsts = xdst(l, b, t, last)
            for i_, (dst, psl) in enumerate(dsts):
                if t < 2:
                    wk = [("cr", b, t)]
                elif len(dsts) == 1:
                    wk = [("xr", b, t - 2, j_) for j_ in range(4)]
                else:
                    wk = [("xr", b, t - 2, i_)]
                if last:
                    wk = [("yout", b, t, i_)]
                    out_keys.append(wk[0])
                S.add("sp", lambda e, dst=dst, psl=psl: e.dma_start(out=dst, in_=rt[psl, :]), ["rt"], wk, dma=True)

        segs = [(0, [0, 1], False), (1, [1, 0], True), (0, list(range(2, NT)), False), (1, list(range(NT - 1, 1, -1)), True)]
        for (d, tiles, fin) in segs:
            for t in tiles:
                prep_tile(t, d)
                chk(6)
                for c in ((0, 1) if d == 0 else (1, 0)):
                    run_rr([scan_chunk(t, d, c, h, fin) for h in range(4)])
                chk(7)
                if fin and not (last and t < 2):
                    finalize(t)
                    chk(8)
        full_barrier(["rt", "yTt", "gat"] + XTK(0) + XTK(1))

    try:
      for l in range(nl):
          cur_l[0] = l
          last = l == NL - 1
          layer_prologue(l, last)
          chk(1)
          for b in range(nb):
              batch_body(l, b, last)
    except _Stop:
        pass
    fin_reads = out_keys + [("tap", n) for n in tap_d] + [("xr", b_, t_, i_) for b_ in range(nb) for t_ in range(16) for i_ in range(4)] + [("cr", b_, t_) for b_ in range(nb) for t_ in range(2)]
    S.add("pool", lambda e: e.memset(sm[:, 60:61], 0.0), fin_reads, [("sm", 60)])
    S.emit(nc, es)
    es.close()
    return nc


def _chunk_cols():
    cols = np.zeros((NCH, 128), np.int64) - 1
    for cc in range(NCH):
        for j in range(128):
            if cc < 6:
                cols[cc, j] = cc * 128 + j
            elif cc == 6:
                cols[cc, j] = 768 + j if j < 16 else -1
            else:
                cols[cc, j] = 784 + (cc - 7) * 128 + j
    return cols


def _consts():
    idx = np.arange(128)
    same = (idx[:, None] // 64) == (idx[None, :] // 64)
    c = np.zeros((128, 9, 128), np.float32)
    c[:, 0] = np.eye(128)
    c[:, 1] = 1.0
    for d in range(2):
        fwd = d == 0
        tri = same & ((idx[:, None] <= idx[None, :]) if fwd else (idx[:, None] >= idx[None, :]))
        allowed = same & ((idx[None, :] >= idx[:, None]) if fwd else (idx[None, :] <= idx[:, None]))
        c[:, 2 + d] = tri
        c[:, 4 + d] = np.where(allowed, 0.0, -30000.0)
        c[:, 6 + d] = allowed & (idx[None, :] != idx[:, None])
    c[:, 8] = same
    return c


def prep_inputs(inp):
    f = lambda a: np.ascontiguousarray(a, dtype=np.float32)
    sh = {}
    w_ada = inp["w_ada"]
    sh["w_ada"] = f(w_ada.reshape(NL, 8, 128, 24, 128).transpose(0, 3, 2, 1, 4))
    sh["b_adaT"] = f(inp["b_ada"].reshape(NL, 24, 128).transpose(0, 2, 1))
    sh["norm_gT"] = f(inp["norm_g"].reshape(NL, 8, 128).transpose(0, 2, 1))
    cols = _chunk_cols()
    w_in = inp["w_in"]
    wpad = np.concatenate([w_in, np.zeros((NL, 1024, 1), np.float32)], axis=2)
    wsel = wpad[:, :, cols.reshape(-1)].reshape(NL, 8, 128, NCH, 128)
    sh["w_in"] = f(wsel.transpose(0, 3, 2, 1, 4))
    sh["qkvw"] = f(inp["qkv_conv_w"].reshape(NL, 3, 6, 128).transpose(0, 3, 2, 1))
    sh["scw"] = f(inp["short_conv_w"].reshape(NL, 3, 2, 128).transpose(0, 3, 2, 1))
    sh["cfw"] = f(inp["conf_conv_w"].reshape(NL, 31, 2, 128).transpose(0, 3, 2, 1))
    cv = np.stack([inp["conf_conv_b"], inp["conf_ln_g"], inp["conf_ln_b"]], axis=1)
    sh["cvec"] = f(cv.reshape(NL, 3, 2, 128).transpose(0, 3, 1, 2))
    rowc = np.concatenate([inp["smlp_ln_g"], inp["smlp_ln_b"], np.tile(inp["gdn_norm_g"], (1, 4)),
                           inp["a_log"].reshape(NL, 8), inp["dt_bias"].reshape(NL, 8)], axis=1)
    sh["rowc"] = f(np.broadcast_to(rowc[:, None, :], (NL, 128, 784)))
    sh["wsT"] = f(inp["smlp_w"].transpose(0, 3, 1, 2))
    bs = inp["smlp_b"].reshape(NL, 2, 2, 1, 128)
    sh["bsT"] = f(np.broadcast_to(bs, (NL, 2, 2, 64, 128)).reshape(NL, 2, 128, 128).transpose(0, 2, 1, 3))
    sh["w_out"] = f(inp["w_out"].reshape(NL, 8, 128, 1024).transpose(0, 2, 1, 3))
    sh["final_gB"] = f(np.broadcast_to(inp["final_g"][None, :], (128, 1024)))
    sh["consts"] = _consts()
    maps = []
    for c in range(8):
        m = dict(sh)
        m["x"] = f(inp["x"][2 * c:2 * c + 2])
        m["ctx"] = f(inp["ctx"][2 * c:2 * c + 2])
        cc = np.stack([inp["c"][2 * c], inp["c"][2 * c + 1], inp["c_ctx"]], axis=1)
        m["ccat"] = f(cc.reshape(8, 128, 3).transpose(1, 0, 2))
        maps.append(m)
    return maps


def kernel(**inputs):
    inputs = {k: np.asarray(v) for k, v in inputs.items()}
    maps = prep_inputs(inputs)
    nc = build()
    res = run_bass_kernel_spmd(nc, maps, core_ids=list(range(8)))
    return np.concatenate([r["y"] for r in res.results], axis=0).astype(np.float32)
```
